# Optimizing a Trainium2 kernel written in Bass

```python
import jax
import jax.numpy as jnp
from jax import lax
import numpy as np

D_MODEL = 1024
BATCH = 32
SEQ = 2048
DEPTH = 4

CHUNK = 64
RET_HEADS = 4
RET_HD = 64
RET_W = RET_HEADS * RET_HD
RWKV_HEADS = 6
RWKV_HD = 64
RWKV_W = RWKV_HEADS * RWKV_HD
MLSTM_HEADS = 4
MLSTM_HD = 96
MLSTM_W = MLSTM_HEADS * MLSTM_HD
D_MIX = RET_W + RWKV_W + MLSTM_W
RWKV_DECAY_LORA = 64
RWKV_ICL_LORA = 64
RWKV_GATE_LORA = 128
MLSTM_CONV = 4
D_FF = ((8 * D_MODEL + 3 * 256 - 1) // (3 * 256)) * 256
ROPE_BASE = 10000.0
RMS_EPS = 1e-6
GN_EPS = 1e-5

RET_SIZES = (RET_W, RET_W, RET_W, RET_W)
RWKV_SIZES = (RWKV_W, RWKV_W, RWKV_W, RWKV_DECAY_LORA, RWKV_ICL_LORA, RWKV_GATE_LORA)
MLSTM_SIZES = (2 * MLSTM_W, MLSTM_W, MLSTM_W, MLSTM_HEADS, MLSTM_HEADS)
RET_IN = sum(RET_SIZES)
RWKV_IN = sum(RWKV_SIZES)
MLSTM_IN = sum(MLSTM_SIZES)
D_IN = RET_IN + RWKV_IN + MLSTM_IN

kernel_name = 'hybrid_ret_rwkv7_mlstm_adaln_trunk'

F32 = jnp.float32


def _split(x, sizes):
    return jnp.split(x, [int(s) for s in np.cumsum(sizes)[:-1]], axis=-1)


def rms_norm(x, gain):
    xf = x.astype(F32)
    y = xf * lax.rsqrt(jnp.mean(xf * xf, axis=-1, keepdims=True) + RMS_EPS)
    return (y * gain.astype(F32)).astype(x.dtype)


def head_norm(x, gain, n_heads):
    b, t, w = x.shape
    xh = x.reshape(b, t, n_heads, w // n_heads)
    mu = jnp.mean(xh, axis=-1, keepdims=True)
    xc = xh - mu
    var = jnp.mean(xc * xc, axis=-1, keepdims=True)
    return (xc * lax.rsqrt(var + GN_EPS)).reshape(b, t, w) * gain.astype(F32)


def rotary(x):
    t, d = x.shape[1], x.shape[-1]
    inv = ROPE_BASE ** (-jnp.arange(0, d, 2, dtype=F32) / d)
    ang = jnp.arange(t, dtype=F32)[:, None] * inv[None, :]
    cos = jnp.cos(ang)[None, :, None, :]
    sin = jnp.sin(ang)[None, :, None, :]
    x1, x2 = jnp.split(x, 2, axis=-1)
    return jnp.concatenate([x1 * cos - x2 * sin, x1 * sin + x2 * cos], axis=-1)


def to_chunks(x):
    b, t, h = x.shape[:3]
    x = x.reshape((b, t // CHUNK, CHUNK, h) + x.shape[3:])
    return jnp.transpose(x, (1, 0, 3, 2) + tuple(range(4, x.ndim)))


def from_chunks(y):
    nc, b, h, l, d = y.shape
    return jnp.transpose(y, (1, 0, 3, 2, 4)).reshape(b, nc * l, h * d)


def token_shift(x, mu):
    xprev = jnp.pad(x, ((0, 0), (1, 0), (0, 0)))[:, :-1]
    return x + (xprev - x) * mu


def causal_conv(x, w, b):
    t = x.shape[1]
    xp = jnp.pad(x, ((0, 0), (MLSTM_CONV - 1, 0), (0, 0)))
    y = b + xp[:, 0:t] * w[0]
    for j in range(1, MLSTM_CONV):
        y = y + xp[:, j:j + t] * w[j]
    return y


def retention(q, k, v):
    b, t, h, d = q.shape
    log_g = jnp.log1p(-jnp.exp2(-5.0 - jnp.arange(h, dtype=F32)))
    idx = jnp.arange(CHUNK, dtype=F32)
    diff = idx[:, None] - idx[None, :]
    intra_decay = jnp.where(diff >= 0, jnp.exp(log_g[:, None, None] * jnp.maximum(diff, 0.0)), 0.0)
    q_decay = jnp.exp(log_g[:, None] * (idx + 1.0))[None, :, :, None]
    k_decay = jnp.exp(log_g[:, None] * (CHUNK - 1.0 - idx))[None, :, :, None]
    chunk_decay = jnp.exp(log_g * CHUNK)[None, :, None, None]
    k = k * (d ** -0.5)

    def step(state, inp):
        qc, kc, vc = inp
        s = jnp.einsum('bhld,bhmd->bhlm', qc, kc) * intra_decay
        y = jnp.einsum('bhlm,bhme->bhle', s, vc) + jnp.einsum('bhld,bhde->bhle', qc, state) * q_decay
        state = state * chunk_decay + jnp.einsum('bhld,bhle->bhde', kc * k_decay, vc)
        return state, y

    init = jnp.zeros((b, h, d, d), F32)
    _, y = lax.scan(step, init, (to_chunks(q), to_chunks(k), to_chunks(v)))
    return from_chunks(y)


def rwkv7_scan(r, decay, k, v, a, kk):
    b, t, h, n = r.shape

    def step(S, inp):
        r_t, w_t, k_t, v_t, a_t, kk_t = inp
        sa = jnp.einsum('bhij,bhj->bhi', S, -kk_t)
        S = S * w_t[:, :, None, :] + sa[..., None] * (kk_t * a_t)[:, :, None, :] + v_t[..., None] * k_t[:, :, None, :]
        return S, jnp.einsum('bhij,bhj->bhi', S, r_t)

    xs = tuple(jnp.moveaxis(z, 1, 0) for z in (r, decay, k, v, a, kk))
    _, y = lax.scan(step, jnp.zeros((b, h, n, n), F32), xs)
    return jnp.moveaxis(y, 0, 1)


def mlstm_chunkwise(q, k, v, i_pre, log_f):
    b, t, h, d = q.shape
    k = k * (d ** -0.5)
    causal = jnp.tril(jnp.ones((CHUNK, CHUNK), dtype=bool))

    def step(carry, inp):
        C, n, m = carry
        qc, kc, vc, ic, fc = inp
        bcum = jnp.cumsum(fc, axis=-1)
        a_inter = bcum + m[..., None]
        D = bcum[..., :, None] - bcum[..., None, :] + ic[..., None, :]
        D = jnp.where(causal, D, -jnp.inf)
        m_t = jnp.maximum(a_inter, jnp.max(D, axis=-1))
        inter_w = jnp.exp(a_inter - m_t)
        s = jnp.einsum('bhld,bhmd->bhlm', qc, kc) * jnp.exp(D - m_t[..., None])
        num = jnp.einsum('bhlm,bhme->bhle', s, vc) + inter_w[..., None] * jnp.einsum('bhld,bhde->bhle', qc, C)
        den = jnp.sum(s, axis=-1) + inter_w * jnp.einsum('bhld,bhd->bhl', qc, n)
        hc = num / jnp.maximum(jnp.abs(den), jnp.exp(-m_t))[..., None]
        b_last = bcum[..., -1]
        g_s = b_last[..., None] - bcum + ic
        m_new = jnp.maximum(b_last + m, jnp.max(g_s, axis=-1))
        ws = jnp.exp(g_s - m_new[..., None])[..., None]
        carry_decay = jnp.exp(b_last + m - m_new)
        C = carry_decay[..., None, None] * C + jnp.einsum('bhld,bhle->bhde', kc * ws, vc)
        n = carry_decay[..., None] * n + jnp.sum(kc * ws, axis=2)
        return (C, n, m_new), hc

    init = (jnp.zeros((b, h, d, d), F32), jnp.zeros((b, h, d), F32), jnp.zeros((b, h), F32))
    _, y = lax.scan(step, init, (to_chunks(q), to_chunks(k), to_chunks(v), to_chunks(i_pre), to_chunks(log_f)))
    return from_chunks(y)


def retention_group(cols, gain):
    b, t, _ = cols.shape
    q, k, v, g = _split(cols.astype(F32), RET_SIZES)
    heads = lambda z: z.reshape(b, t, RET_HEADS, RET_HD)
    y = retention(rotary(heads(q)), rotary(heads(k)), heads(v))
    return head_norm(y, gain, RET_HEADS) * jax.nn.silu(g)


def rwkv_group(cols, gain, mu, w0, w2, a0, a2, g2, k_k, k_a, r_k):
    b, t, _ = cols.shape
    cols = token_shift(cols.astype(F32), mu.astype(F32))
    r, k, v, wl, al, gl = _split(cols, RWKV_SIZES)
    log_w = jax.nn.log_sigmoid(w0.astype(F32) + jnp.tanh(wl) @ w2.astype(F32)) - 0.5
    decay = jnp.exp(-jnp.exp(log_w))
    a = jax.nn.sigmoid(a0.astype(F32) + al @ a2.astype(F32))
    g = jax.nn.sigmoid(gl) @ g2.astype(F32)
    heads = lambda z: z.reshape(b, t, RWKV_HEADS, RWKV_HD)
    r, k, v, decay, a = heads(r), heads(k), heads(v), heads(decay), heads(a)
    kk = k * k_k.astype(F32).reshape(RWKV_HEADS, RWKV_HD)
    kk = kk / jnp.maximum(jnp.sqrt(jnp.sum(kk * kk, axis=-1, keepdims=True)), 1e-12)
    k = k * (1.0 + (a - 1.0) * k_a.astype(F32).reshape(RWKV_HEADS, RWKV_HD))
    y = rwkv7_scan(r, decay, k, v, a, kk).reshape(b, t, RWKV_W)
    bonus = (jnp.sum(r * k * r_k.astype(F32), axis=-1, keepdims=True) * v).reshape(b, t, RWKV_W)
    return (head_norm(y, gain, RWKV_HEADS) + bonus) * g


def mlstm_group(cols, gain, conv_w, conv_b, i_b, f_b):
    b, t, _ = cols.shape
    qk, v, o, i_pre, f_pre = _split(cols.astype(F32), MLSTM_SIZES)
    qk = jax.nn.silu(causal_conv(qk, conv_w.astype(F32), conv_b.astype(F32)))
    q, k = jnp.split(qk, 2, axis=-1)
    heads = lambda z: z.reshape(b, t, MLSTM_HEADS, MLSTM_HD)
    h = mlstm_chunkwise(heads(q), heads(k), heads(v), i_pre + i_b.astype(F32),
                        jax.nn.log_sigmoid(f_pre + f_b.astype(F32)))
    return head_norm(h, gain, MLSTM_HEADS) * jax.nn.sigmoid(o)


def setup_inputs(seed: int = 0) -> dict:
    key = jax.random.key(seed)
    ks = jax.random.split(key, 26)
    nrm = lambda k, shape, scale: jax.random.normal(k, shape, F32) * scale
    L, D = DEPTH, D_MODEL
    return {
        'x': nrm(ks[0], (BATCH, SEQ, D), 1.0),
        'c': nrm(ks[1], (BATCH, D), 1.0),
        'ada_w': nrm(ks[2], (L, D, 6 * D), 0.5 * D ** -0.5),
        'ada_b': nrm(ks[3], (L, 6 * D), 0.02),
        'norm_mix': 1.0 + nrm(ks[4], (L, D), 0.02),
        'norm_ffn': 1.0 + nrm(ks[5], (L, D), 0.02),
        'w_in': nrm(ks[6], (L, D, D_IN), D ** -0.5),
        'mix_gn': 1.0 + nrm(ks[7], (L, D_MIX), 0.02),
        'rwkv_mu': jax.random.uniform(ks[8], (L, RWKV_IN), F32, 0.0, 1.0),
        'rwkv_w0': jnp.linspace(-6.0, -1.0, RWKV_W, dtype=F32)[None, :] + nrm(ks[9], (L, RWKV_W), 0.1),
        'rwkv_w2': nrm(ks[10], (L, RWKV_DECAY_LORA, RWKV_W), 0.5 * RWKV_DECAY_LORA ** -0.5),
        'rwkv_a0': nrm(ks[11], (L, RWKV_W), 0.1),
        'rwkv_a2': nrm(ks[12], (L, RWKV_ICL_LORA, RWKV_W), 0.5 * RWKV_ICL_LORA ** -0.5),
        'rwkv_g2': nrm(ks[13], (L, RWKV_GATE_LORA, RWKV_W), RWKV_GATE_LORA ** -0.5),
        'rwkv_k_k': 1.0 + nrm(ks[14], (L, RWKV_W), 0.05),
        'rwkv_k_a': 1.0 + nrm(ks[15], (L, RWKV_W), 0.05),
        'rwkv_r_k': nrm(ks[16], (L, RWKV_HEADS, RWKV_HD), 0.1),
        'mlstm_conv_w': nrm(ks[17], (L, MLSTM_CONV, 2 * MLSTM_W), MLSTM_CONV ** -0.5),
        'mlstm_conv_b': nrm(ks[18], (L, 2 * MLSTM_W), 0.02),
        'mlstm_i_b': nrm(ks[19], (L, MLSTM_HEADS), 0.1),
        'mlstm_f_b': jnp.linspace(3.0, 6.0, MLSTM_HEADS, dtype=F32)[None, :] + nrm(ks[20], (L, MLSTM_HEADS), 0.1),
        'w_out': nrm(ks[21], (L, D_MIX, D), D_MIX ** -0.5),
        'ffn_w_in': nrm(ks[22], (L, D, 2 * D_FF), D ** -0.5),
        'ffn_w_out': nrm(ks[23], (L, D_FF, D), D_FF ** -0.5),
        'final_norm': 1.0 + nrm(ks[24], (D,), 0.02),
    }


def reference(x, c, ada_w, ada_b, norm_mix, norm_ffn, w_in, mix_gn, rwkv_mu, rwkv_w0, rwkv_w2,
              rwkv_a0, rwkv_a2, rwkv_g2, rwkv_k_k, rwkv_k_a, rwkv_r_k, mlstm_conv_w, mlstm_conv_b,
              mlstm_i_b, mlstm_f_b, w_out, ffn_w_in, ffn_w_out, final_norm):
    cond = jax.nn.silu(c)
    for l in range(DEPTH):
        mod = cond @ ada_w[l] + ada_b[l]
        sh_m, sc_m, g_m, sh_f, sc_f, g_f = [z[:, None, :] for z in jnp.split(mod, 6, axis=-1)]
        h = rms_norm(x, norm_mix[l]) * (1.0 + sc_m) + sh_m
        proj = h @ w_in[l]
        ret_cols, rwkv_cols, mlstm_cols = _split(proj, (RET_IN, RWKV_IN, MLSTM_IN))
        gn_ret, gn_rwkv, gn_mlstm = _split(mix_gn[l], (RET_W, RWKV_W, MLSTM_W))
        y_ret = retention_group(ret_cols, gn_ret)
        y_rwkv = rwkv_group(rwkv_cols, gn_rwkv, rwkv_mu[l], rwkv_w0[l], rwkv_w2[l], rwkv_a0[l], rwkv_a2[l],
                            rwkv_g2[l], rwkv_k_k[l], rwkv_k_a[l], rwkv_r_k[l])
        y_mlstm = mlstm_group(mlstm_cols, gn_mlstm, mlstm_conv_w[l], mlstm_conv_b[l], mlstm_i_b[l], mlstm_f_b[l])
        mixed = jnp.concatenate([y_ret, y_rwkv, y_mlstm], axis=-1).astype(x.dtype)
        x = x + g_m * (mixed @ w_out[l])
        h = rms_norm(x, norm_ffn[l]) * (1.0 + sc_f) + sh_f
        gate, up = jnp.split(h @ ffn_w_in[l], 2, axis=-1)
        x = x + g_f * ((jax.nn.silu(gate) * up) @ ffn_w_out[l])
    return rms_norm(x, final_norm)
```

```python
import math
from contextlib import ExitStack
import numpy as np
import ml_dtypes
import concourse.bass as bass
import concourse.mybir as mybir
from concourse.bass_utils import run_bass_kernel_spmd

F32 = mybir.dt.float32
BF16 = mybir.dt.bfloat16
ALU = mybir.AluOpType
AF = mybir.ActivationFunctionType
AX = mybir.AxisListType

D = 1024
DFF = 2816
NFB = 22
RMS_EPS = 1e-6
GN_EPS = 1e-5
TT = 128
BL = 128


class Unit:
    __slots__ = ("name", "w", "r", "excl")

    def __init__(self, name):
        self.name = name
        self.w = None
        self.r = []
        self.excl = False


class V:
    __slots__ = ("unit", "ap")

    def __init__(self, unit, ap):
        self.unit = unit
        self.ap = ap

    def rearrange(self, pat, **kw):
        return V(self.unit, self.ap.rearrange(pat, **kw))

    def __getitem__(self, idx):
        return V(self.unit, self.ap[idx])


class TileH:
    def __init__(self, handle, name):
        self.h = handle
        self.unit = Unit(name)
        self.name = name

    def __getitem__(self, idx):
        return V(self.unit, self.h[idx])

    def sub(self, key):
        t = TileH.__new__(TileH)
        t.h = self.h
        t.unit = Unit(self.name + str(key))
        t.name = t.unit.name
        return t


ENGS = ("pe", "dve", "act", "pool", "sp")
INLINE_WAIT = True
NDS = 24


class Prog:
    def __init__(self, nc, es):
        self.nc = nc
        self.es = es
        self.ins = {e: [] for e in ENGS}
        self.ndma = 0
        self.dma_tokens = []
        self.psf = []
        self.psb = []
        self.ipf = 0
        self.ipb = 0

    def sb(self, name, shape, dt):
        h = self.es.enter_context(self.nc.sbuf_tensor("sb_" + name, list(shape), dt))
        return TileH(h, name)

    def alloc_psum(self):
        for i in range(8):
            h = self.es.enter_context(self.nc.psum_tensor("psf%d" % i, [128, 512], F32))
            self.psf.append(TileH(h, "psf%d" % i))
            self.psf[-1].unit.excl = True

    def ps(self):
        t = self.psf[self.ipf % 8]
        self.ipf += 1
        return t

    def pb(self):
        return self.ps()

    def _rec(self, eng, fn, reads, writes, dma=False, force=False):
        if eng == "pool" and not dma and not force:
            eng = "dve"
        deps = set()
        for v in reads:
            if isinstance(v, V) and v.unit.w is not None:
                deps.add(v.unit.w)
            if isinstance(v, V) and v.unit.excl:
                deps.update(v.unit.r)
        for v in writes:
            if isinstance(v, V):
                if v.unit.w is not None:
                    deps.add(v.unit.w)
                deps.update(v.unit.r)
        idx = len(self.ins[eng])
        if dma:
            did = self.ndma
            self.ndma += 1
            tok = ("dma", did)
            if did >= NDS:
                deps.add(("dma", did - NDS))
        else:
            did = None
            tok = (eng, idx)
        self.ins[eng].append({"fn": fn, "deps": deps, "sig": False, "dma": did})
        for v in reads:
            if isinstance(v, V):
                v.unit.r.append(tok)
        for v in writes:
            if isinstance(v, V):
                v.unit.w = tok
                v.unit.r = []
        return tok

    @staticmethod
    def _ap(v):
        return v.ap if isinstance(v, V) else v

    def mm(self, out, lhsT, rhs, start=True, stop=True):
        o, l, r = self._ap(out), self._ap(lhsT), self._ap(rhs)
        self._rec("pe", lambda e: e.matmul(o, l, r, start=start, stop=stop), [lhsT, rhs], [out])

    def tr(self, out, in_, ident):
        self.mm(out, in_, ident)

    def act(self, out, in_, func, bias=0.0, scale=1.0, eng="act"):
        o, i = self._ap(out), self._ap(in_)
        b, s = self._ap(bias), self._ap(scale)
        rd = [in_] + [x for x in (bias, scale) if isinstance(x, V)]
        self._rec("act", lambda e: e.activation(out=o, in_=i, func=func, bias=b, scale=s), rd, [out])

    def tt(self, out, in0, in1, op, eng="dve"):
        o, a, b = self._ap(out), self._ap(in0), self._ap(in1)
        self._rec(eng, lambda e: e.tensor_tensor(out=o, in0=a, in1=b, op=op), [in0, in1], [out])

    def ts(self, out, in0, s1, op0, s2=None, op1=None, eng="dve"):
        o, a = self._ap(out), self._ap(in0)
        x1, x2 = self._ap(s1), self._ap(s2)
        rd = [in0] + [x for x in (s1, s2) if isinstance(x, V)]
        if op1 is None:
            self._rec(eng, lambda e: e.tensor_scalar(out=o, in0=a, scalar1=x1, scalar2=None, op0=op0), rd, [out])
        else:
            self._rec(eng, lambda e: e.tensor_scalar(out=o, in0=a, scalar1=x1, scalar2=x2, op0=op0, op1=op1), rd, [out])

    def stt(self, out, in0, scalar, in1, op0, op1, eng="dve"):
        o, a, b, s = self._ap(out), self._ap(in0), self._ap(in1), self._ap(scalar)
        rd = [in0, in1] + ([scalar] if isinstance(scalar, V) else [])
        self._rec(eng, lambda e: e.scalar_tensor_tensor(out=o, in0=a, scalar=s, in1=b, op0=op0, op1=op1), rd, [out])

    def cp(self, out, in_, eng="dve"):
        o, i = self._ap(out), self._ap(in_)
        if eng == "act":
            self._rec("act", lambda e: e.activation(out=o, in_=i, func=AF.Copy), [in_], [out])
        else:
            self._rec(eng, lambda e: e.tensor_copy(out=o, in_=i), [in_], [out])

    def memset(self, out, val, eng="dve", force=False):
        o = self._ap(out)
        self._rec(eng, lambda e: e.memset(o, val), [], [out], force=force)

    def scan(self, out, d0, d1, init, op0, op1):
        o, a, b, i = self._ap(out), self._ap(d0), self._ap(d1), self._ap(init)
        rd = [d0, d1] + ([init] if isinstance(init, V) else [])
        self._rec("dve", lambda e: e.tensor_tensor_scan(out=o, data0=a, data1=b, initial=i, op0=op0, op1=op1), rd, [out])

    def red(self, out, in_, op=ALU.add):
        o, i = self._ap(out), self._ap(in_)
        self._rec("dve", lambda e: e.tensor_reduce(out=o, in_=i, axis=AX.X, op=op), [in_], [out])

    def recip(self, out, in_):
        o, i = self._ap(out), self._ap(in_)
        self._rec("dve", lambda e: e.reciprocal(out=o, in_=i), [in_], [out])

    def dma(self, q, out, in_):
        o, i = self._ap(out), self._ap(in_)
        tok = self._rec(q, lambda e: e.dma_start(out=o, in_=i), [in_], [out], dma=True)
        self.dma_tokens.append(tok)
        return tok

    def emit(self, final_tokens):
        nc = self.nc
        es = self.es
        sems = {e: es.enter_context(nc.semaphore("s_" + e)) for e in ENGS}
        dsem = [es.enter_context(nc.semaphore("d%d" % i)) for i in range(NDS)]
        self.ins["sp"].append({"fn": None, "deps": set(final_tokens), "sig": False, "dma": None})
        for e in ENGS:
            for idx, rec in enumerate(self.ins[e]):
                for tok in rec["deps"]:
                    if tok[0] == "dma":
                        continue
                    pe, pidx = tok
                    if pe == e and (e == "pe" or idx - pidx > 3):
                        continue
                    self.ins[pe][pidx]["sig"] = True
        cnt = {}
        for e in ENGS:
            c = 0
            arr = []
            for rec in self.ins[e]:
                if rec["sig"]:
                    c += 1
                arr.append(c)
            cnt[e] = arr
        block = es.enter_context(nc.Block())

        def emit_engine(e, eng):
            seen = {x: 0 for x in ENGS}
            seend = [0] * NDS
            for idx, rec in enumerate(self.ins[e]):
                need = {}
                needd = {}
                for tok in rec["deps"]:
                    if tok[0] == "dma":
                        did = tok[1]
                        slot = did % NDS
                        val = 16 * (did // NDS + 1)
                        if val > seend[slot]:
                            needd[slot] = max(needd.get(slot, 0), val)
                    else:
                        pe, pidx = tok
                        if pe == e and (e == "pe" or idx - pidx > 3):
                            continue
                        val = cnt[pe][pidx]
                        if val > seen[pe]:
                            need[pe] = max(need.get(pe, 0), val)
                waits = []
                for pe, val in need.items():
                    waits.append((sems[pe], val))
                    seen[pe] = val
                for slot, val in needd.items():
                    waits.append((dsem[slot], val))
                    seend[slot] = val
                inline = None
                if rec["fn"] is not None and waits and INLINE_WAIT:
                    inline = waits.pop()
                for sm, val in waits:
                    eng.wait_ge(sm, val)
                if rec["fn"] is None:
                    continue
                inst = rec["fn"](eng)
                if inline is not None:
                    inst._wait_ge(inline[0], inline[1])
                if rec["dma"] is not None:
                    inst.then_inc(dsem[rec["dma"] % NDS], 16)
                elif rec["sig"]:
                    inst.then_inc(sems[e], 1)

        block.tensor(lambda eng: emit_engine("pe", eng))
        block.vector(lambda eng: emit_engine("dve", eng))
        block.scalar(lambda eng: emit_engine("act", eng))
        block.gpsimd(lambda eng: emit_engine("pool", eng))
        block.sync(lambda eng: emit_engine("sp", eng))


RET_OFF = 0
RWKV_OFF = 1024
ML_OFF = 1024 + 1408


def _swap_half(cols):
    out = []
    for h in range(0, len(cols), 64):
        blk = cols[h:h + 64]
        out += blk[32:] + blk[:32]
    return out


def colblocks_B():
    blocks = []
    rq = list(range(0, 256))
    rk = list(range(256, 512))
    rg = list(range(768, 1024))
    for p in range(2):
        blocks.append(("rqn%d" % p, rq[128 * p:128 * p + 128]))
    for p in range(2):
        blocks.append(("rqs%d" % p, _swap_half(rq)[128 * p:128 * p + 128]))
    for p in range(2):
        blocks.append(("rkn%d" % p, rk[128 * p:128 * p + 128]))
    for p in range(2):
        blocks.append(("rks%d" % p, _swap_half(rk)[128 * p:128 * p + 128]))
    for p in range(2):
        blocks.append(("rg%d" % p, rg[128 * p:128 * p + 128]))
    o = RWKV_OFF
    for nm, base in (("wr", 0), ("wk", 384), ("wv", 768)):
        for p in range(3):
            blocks.append(("%s%d" % (nm, p), list(range(o + base + 128 * p, o + base + 128 * p + 128))))
    blocks.append(("wwa", list(range(o + 1152, o + 1152 + 128))))
    blocks.append(("wgl", list(range(o + 1280, o + 1408))))
    o = ML_OFF
    for h in range(4):
        blocks.append(("mq%d" % h, list(range(o + 96 * h, o + 96 * h + 96))))
    for h in range(4):
        blocks.append(("mk%d" % h, list(range(o + 384 + 96 * h, o + 384 + 96 * h + 96))))
    blocks.append(("mi", list(range(o + 1536, o + 1540))))
    blocks.append(("mf", list(range(o + 1540, o + 1544))))
    return blocks


COLS_A = list(range(512, 768)) + list(range(ML_OFF + 768, ML_OFF + 1152)) + list(range(ML_OFF + 1152, ML_OFF + 1536))


def _kmajor(w):
    C = w.shape[1]
    return np.ascontiguousarray(w.reshape(8, 128, C).transpose(1, 0, 2))


def pcol(v, n):
    return np.ascontiguousarray(np.asarray(v, np.float32).reshape(n, 128).T)


def build_consts(T):
    c = {}
    c["ident_f"] = np.eye(128, dtype=np.float32)
    c["ident_b"] = np.eye(128, dtype=np.float32).astype(ml_dtypes.bfloat16)
    c["onesN"] = np.full((128, 128), 1.0 / 1024.0, np.float32).astype(ml_dtypes.bfloat16)
    blk = np.zeros((128, 128), np.float32)
    blk[:64, :64] = 1.0
    blk[64:, 64:] = 1.0
    c["blk1"] = blk
    c["blk64"] = blk / 64.0
    inv = 10000.0 ** (-np.arange(0, 64, 2, dtype=np.float64) / 64.0)
    t = np.arange(T, dtype=np.float64)
    ang = t[None, :] * inv[:, None]
    cos = np.cos(ang)
    sin = np.sin(ang)
    cosT = np.concatenate([cos, cos, cos, cos], 0)
    sinT = np.concatenate([-sin, sin, -sin, sin], 0)
    rope = np.ascontiguousarray(np.stack([cosT, sinT], 1)).astype(np.float32)
    lg = np.log1p(-np.exp2(-5.0 - np.arange(4, dtype=np.float64)))
    idx = np.arange(BL, dtype=np.float64)
    diff = idx[None, :] - idx[:, None]
    md = np.zeros((128, 4, 128), np.float64)
    for h in range(4):
        md[:, h, :] = np.where(diff >= 0, np.exp(lg[h] * np.maximum(diff, 0)), 0.0) * 0.125
    c["ret_mdT"] = md.astype(np.float32).reshape(128, 512)
    qd = np.zeros((128, 2, TT), np.float64)
    for p in range(2):
        for hh in range(2):
            h = 2 * p + hh
            row = np.exp(lg[h] * (idx + 1.0))
            qd[64 * hh:64 * hh + 64, p, :] = np.tile(row, TT // BL)[None, :]
    c["ret_qd"] = qd.astype(np.float32)
    kd = np.zeros((128, 4, 64), np.float64)
    for h in range(4):
        kd[:, h, :] = np.exp(lg[h] * (BL - 1.0 - idx))[:, None] * 0.125
    c["ret_kd"] = kd.astype(np.float32).reshape(128, 256)
    cd = np.zeros((128, 2, 128), np.float64)
    for p in range(2):
        for hh in range(2):
            cd[64 * hh:64 * hh + 64, p, :] = np.exp(lg[2 * p + hh] * BL)
    c["ret_cd"] = cd.astype(np.float32).reshape(128, 256)
    up_inc = (idx[:, None] <= idx[None, :]).astype(np.float32)
    up_str = (idx[:, None] < idx[None, :]).astype(np.float32)
    lo_str = (idx[:, None] > idx[None, :]).astype(np.float32)
    c["m_ml4"] = (np.ascontiguousarray(np.broadcast_to(up_inc[:, None, :], (128, 4, 128))) * (96.0 ** -0.5)).astype(np.float32).reshape(128, 512)
    c["m_rw"] = np.ascontiguousarray(np.stack([up_str, up_inc, up_str, up_inc], 1)).astype(np.float32).reshape(128, 512)
    c["m_lo"] = lo_str
    rst = np.ones((128, TT), np.float32)
    rst[:, ::BL] = 0.0
    c["rst"] = rst
    c["ones_r"] = np.ones((128, TT), np.float32)
    ep = np.zeros((128, 2), np.float32)
    ep[:, 0] = RMS_EPS
    ep[:, 1] = GN_EPS
    c["epsc"] = ep
    sel = np.zeros((128, 4, 128), np.float32)
    for h in range(4):
        sel[h, h, :] = 1.0
    c["sel4"] = sel
    return c, rope


def build_layer_inputs(inp, L):
    out = {}
    blocks = colblocks_B()
    NB = len(blocks)
    w_in = np.asarray(inp["w_in"], np.float32)
    WB = np.zeros((L, NB, 128, 8, 128), np.float32)
    for l in range(L):
        for b, (nm, cols) in enumerate(blocks):
            WB[l, b, :, :, :len(cols)] = _kmajor(w_in[l][:, cols])
    out["WB"] = WB
    WA = np.zeros((L, 128, 8, 1024), np.float32)
    for l in range(L):
        WA[l] = _kmajor(w_in[l][:, COLS_A])
    out["WA"] = WA
    w_out = np.asarray(inp["w_out"], np.float32)
    WO = np.zeros((L, 8, 128, 8, 128), np.float32)
    for l in range(L):
        km = _kmajor(w_out[l])
        for oc in range(8):
            WO[l, oc] = km[:, :, oc * 128:(oc + 1) * 128]
    out["WO"] = WO
    f1 = np.asarray(inp["ffn_w_in"], np.float32)
    W1 = np.zeros((L, NFB, 128, 8, 256), np.float32)
    for l in range(L):
        km = _kmajor(f1[l])
        for fb in range(NFB):
            W1[l, fb, :, :, 0:128] = km[:, :, fb * 128:(fb + 1) * 128]
            W1[l, fb, :, :, 128:256] = km[:, :, DFF + fb * 128:DFF + (fb + 1) * 128]
    out["W1"] = W1
    f2 = np.asarray(inp["ffn_w_out"], np.float32)
    W2 = np.zeros((L, 8, 128, NFB, 128), np.float32)
    for l in range(L):
        km = np.ascontiguousarray(f2[l].reshape(NFB, 128, 1024).transpose(1, 0, 2))
        for oc in range(8):
            W2[l, oc] = km[:, :, oc * 128:(oc + 1) * 128]
    out["W2"] = W2
    ada = np.asarray(inp["ada_w"], np.float32)
    ADA = np.zeros((L, 48, 128, 8, 128), np.float32)
    for l in range(L):
        km = _kmajor(ada[l])
        for cb in range(48):
            ADA[l, cb] = km[:, :, cb * 128:(cb + 1) * 128]
    out["ADA"] = ADA
    pv = np.zeros((L, 128, 192), np.float32)
    rwl = np.zeros((L, 128, 1152), np.float32)
    for l in range(L):
        j = 0
        pv[l, :, 0:48] = pcol(inp["ada_b"][l], 48)
        pv[l, :, 48:56] = pcol(inp["norm_mix"][l], 8)
        pv[l, :, 56:64] = pcol(inp["norm_ffn"][l], 8)
        gn = np.asarray(inp["mix_gn"][l], np.float32)
        pv[l, :, 64:66] = pcol(gn[0:256], 2)
        pv[l, :, 66:69] = pcol(gn[256:640], 3)
        mu = np.asarray(inp["rwkv_mu"][l], np.float32)
        pv[l, :, 69:80] = pcol(mu, 11)
        pv[l, :, 80:83] = pcol(inp["rwkv_w0"][l], 3)
        pv[l, :, 83:86] = pcol(inp["rwkv_a0"][l], 3)
        pv[l, :, 86:89] = pcol(inp["rwkv_k_k"][l], 3)
        pv[l, :, 89:92] = pcol(inp["rwkv_k_a"][l], 3)
        pv[l, :, 92:95] = pcol(np.asarray(inp["rwkv_r_k"][l], np.float32).reshape(-1), 3)
        cw = np.asarray(inp["mlstm_conv_w"][l], np.float32)
        cb_ = np.asarray(inp["mlstm_conv_b"][l], np.float32)
        for b8 in range(8):
            ch = slice(96 * b8, 96 * b8 + 96)
            for tap in range(4):
                pv[l, 0:96, 96 + b8 * 5 + tap] = cw[tap, ch]
            pv[l, 0:96, 96 + b8 * 5 + 4] = cb_[ch]
        pv[l, 0:4, 140] = np.asarray(inp["mlstm_i_b"][l], np.float32)
        pv[l, 0:4, 141] = np.asarray(inp["mlstm_f_b"][l], np.float32)
        rwl[l, 0:64, 0:384] = inp["rwkv_w2"][l]
        rwl[l, 64:128, 384:768] = inp["rwkv_a2"][l]
        rwl[l, :, 768:1152] = inp["rwkv_g2"][l]
        pv[l, :, 142:145] = pcol(gn[640:1024], 3)
    out["PV"] = pv
    out["RWL"] = rwl
    out["FNORM"] = pcol(inp["final_norm"], 8)
    return out


def build_program(BPC, T, L, mixers=("ret", "rwkv", "ml"), dbg=()):
    NT = T // TT
    blocks = colblocks_B()
    NB = len(blocks)
    bidx = {nm: i for i, (nm, _) in enumerate(blocks)}
    bM = {nm: 128 for nm, cols in blocks}
    nc = bass.Bass("TRN2", target_bir_lowering=False)
    es = ExitStack()
    P = Prog(nc, es)

    def din(name, shape, dt=F32):
        return nc.dram_tensor(name, list(shape), dt, kind="ExternalInput").ap()

    x_d = din("x", [BPC, 128, 8, T])
    c_d = din("cT", [128, 8, BPC])
    WB_d = din("WB", [L, NB, 128, 8, 128])
    WA_d = din("WA", [L, 128, 8, 1024])
    WO_d = din("WO", [L, 8, 128, 8, 128])
    W1_d = din("W1", [L, NFB, 128, 8, 256])
    W2_d = din("W2", [L, 8, 128, NFB, 128])
    ADA_d = din("ADA", [L, 48, 128, 8, 128])
    PV_d = din("PV", [L, 128, 192])
    RWL_d = din("RWL", [L, 128, 1152])
    FN_d = din("FNORM", [128, 8])
    consts, rope_np = build_consts(T)
    rope_d = din("rope", [128, 2, T])
    cdram = {}
    for k, v in consts.items():
        cdram[k] = din("c_" + k, v.shape, BF16 if v.dtype == ml_dtypes.bfloat16 else F32)
    y_d = nc.dram_tensor("y", [BPC, 128, 8, T], F32, kind="ExternalOutput").ap()
    dbg_d = {}
    WBs = nc.dram_tensor("WBs", [L, NB, 128, 8, 128], BF16, kind="Internal").ap()
    WAs = nc.dram_tensor("WAs", [L, 128, 8, 1024], BF16, kind="Internal").ap()
    WOs = nc.dram_tensor("WOs", [L, 8, 128, 8, 128], BF16, kind="Internal").ap()
    W1s = nc.dram_tensor("W1s", [L, NFB, 128, 8, 256], BF16, kind="Internal").ap()
    W2s = nc.dram_tensor("W2s", [L, 8, 128, NFB, 128], BF16, kind="Internal").ap()

    P.alloc_psum()
    C = {}
    for k, v in consts.items():
        shp = list(v.shape)
        if True:
            C[k] = P.sb("k_" + k, shp, BF16 if v.dtype == ml_dtypes.bfloat16 else F32)
        P.dma("sp", C[k][:], cdram[k])
    cast_tokens = []
    for l in range(L if "nocast" not in mixers else 0):
        for b in range(NB):
            cast_tokens.append(P.dma("pool", WBs[l, b], WB_d[l, b]))
        for kc in range(8):
            cast_tokens.append(P.dma("pool", WAs[l, :, kc, :], WA_d[l, :, kc, :]))
        for oc in range(8):
            cast_tokens.append(P.dma("pool", WOs[l, oc], WO_d[l, oc]))
            cast_tokens.append(P.dma("pool", W2s[l, oc], W2_d[l, oc]))
        for fb in range(NFB):
            cast_tokens.append(P.dma("pool", W1s[l, fb], W1_d[l, fb]))
    scratch_unit = Unit("scratch")
    P.ins["pool"].append({"fn": None, "deps": set(cast_tokens), "sig": False, "dma": None})
    barrier_tok = ("pool", len(P.ins["pool"]) - 1)
    kb = P.sb("kbar", [128, 8], F32)
    P.memset(kb[:, :], 0.0, eng="pool", force=True)
    scratch_unit.w = ("pool", len(P.ins["pool"]) - 1)

    def SV(ap):
        return V(scratch_unit, ap)

    PV = P.sb("PV", [128, L, 192], F32)
    w_raw = P.sb("w_raw", [128, 11, 1 + TT], F32)
    w_sh = P.sb("w_sh", [128, 11, TT], F32)
    RWLf = w_sh[:, 0:9, :]
    RWL = P.sb("RWL", [128, L, 1152], BF16)
    FN = P.sb("FN", [128, 8], F32)
    for l in range(L):
        P.dma("sp", PV[:, l, :], PV_d[l])
        P.dma("sp", RWLf, RWL_d[l].rearrange("p (a b) -> p a b", b=128))
        P.cp(RWL[:, l, :].rearrange("p (a b) -> p a b", b=128), RWLf)
    P.dma("sp", FN[:, :], FN_d)
    omka = P.sb("omka", [128, L, 3], F32)
    for l in range(L):
        P.ts(omka[:, l, :], PV[:, l, 89:92], -1.0, ALU.mult, 1.0, ALU.add)

    cT = P.sb("cT", [128, 8, BPC], F32)
    P.dma("sp", cT[:, :, :], c_d)
    cond = P.sb("cond", [128, 8, BPC], F32)
    P.act(cond[:, :, :], cT[:, :, :], AF.Silu)
    modT = P.sb("modT", [128, L * BPC, 48], F32)
    gmodm = P.sb("gmodm", [128, L * BPC, 8], F32)
    gmodf = P.sb("gmodf", [128, L * BPC, 8], F32)
    adaw = [w_sh[:, 0:8, :], w_raw[:, 0:8, 0:TT]]
    for l in range(L if "noada" not in mixers else 0):
        for cb in range(48):
            wt = adaw[(l * 48 + cb) % 2]
            P.dma("sp", wt[:, :, :], ADA_d[l, cb])
            ps = P.ps()
            for kc in range(8):
                P.mm(ps[:, 0:BPC], wt[:, kc, :], cond[:, kc, :], start=(kc == 0), stop=(kc == 7))
            for b in range(BPC):
                P.act(modT[:, l * BPC + b, cb:cb + 1], ps[:, b:b + 1], AF.Identity, bias=PV[:, l, cb:cb + 1])
        for b in range(BPC):
            lb = l * BPC + b
            P.ts(gmodm[:, lb, :], modT[:, lb, 8:16], 1.0, ALU.add)
            P.tt(gmodm[:, lb, :], gmodm[:, lb, :], PV[:, l, 48:56], ALU.mult)
            P.ts(gmodf[:, lb, :], modT[:, lb, 32:40], 1.0, ALU.add)
            P.tt(gmodf[:, lb, :], gmodf[:, lb, :], PV[:, l, 56:64], ALU.mult)

    xT = [P.sb("xT%d" % i, [128, 8, TT], F32) for i in range(2)]
    ropeb = [P.sb("rope%d" % i, [128, 2, TT], F32) for i in range(1)] * 2
    hT = P.sb("hT", [128, 8, TT], BF16)
    sq = [P.sb("sq%d" % i, [128, TT], BF16) for i in range(2)]
    rstd = P.sb("rstd", [128, TT], F32)
    tmpn = [P.sb("tmpn%d" % i, [128, TT], F32) for i in range(1)] * 2
    wbuf = [P.sb("wbuf%d" % i, [128, 8, 128], BF16) for i in range(2)]
    wA = P.sb("wA", [128, 8, 1024], BF16)
    mixT = P.sb("mixT", [128, 8, TT], BF16)
    w1buf = [P.sb("w1b%d" % i, [128, 8, 256], BF16) for i in range(1)] * 2
    w2buf = [P.sb("w2b%d" % i, [128, NFB, 128], BF16) for i in range(1)] * 2
    actT = P.sb("actT", [128, NFB, TT], BF16)
    sgt = [P.sb("sgt%d" % i, [128, TT], F32) for i in range(1)] * 2
    wcnt = [0]

    def load_wblock(l, nm):
        wt = wbuf[wcnt[0] % 2]
        wcnt[0] += 1
        P.dma("sp", wt[:, :, :], SV(WBs[l, bidx[nm]]))
        return wt

    def projB(l, nm, ps_view_fn):
        wt = load_wblock(l, nm)
        M = bM[nm]
        ps = P.ps()
        for kc in range(8):
            P.mm(ps[0:M, 0:TT], wt[:, kc, 0:M], hT[:, kc, :], start=(kc == 0), stop=(kc == 7))
        return ps

    def rsqrt_eps(dst, src, eps, op=ALU.add):
        P.ts(dst, src, eps, op)
        P.act(dst, dst, AF.Ln)
        P.act(dst, dst, AF.Exp, scale=-0.5)

    def norm_mod(ti, gm_col, sh_col):
        x = xT[ti % 2]
        psm = P.ps()
        for kc in range(8):
            s = sq[kc % 2]
            P.act(s[:, :], x[:, kc, :], AF.Square)
            P.mm(psm[:, 0:TT], C["onesN"][:, :], s[:, :], start=(kc == 0), stop=(kc == 7))
        rsqrt_eps(rstd[:, :], psm[:, 0:TT], RMS_EPS)
        for kc in range(8):
            tm = tmpn[kc % 2]
            P.tt(tm[:, :], x[:, kc, :], rstd[:, :], ALU.mult)
            P.act(hT[:, kc, :], tm[:, :], AF.Identity, bias=sh_col(kc), scale=gm_col(kc))

    r_q = P.sb("r_q", [128, 2, TT], BF16)
    r_qd = P.sb("r_qd", [128, 2, TT], BF16)
    r_k = P.sb("r_k", [128, 2, TT], BF16)
    r_t1 = P.sb("r_t1", [128, TT], F32)
    r_t2 = P.sb("r_t2", [128, TT], F32)
    r_gs = P.sb("r_gs", [128, 2, TT], F32)
    r_S_l = [P.sb("r_S%d" % i, [128, 256], F32) for i in range(L)]
    r_Sb_l = [P.sb("r_Sb%d" % i, [128, 256], BF16) for i in range(L)]
    r_vtok = P.sb("r_vtok", [128, 256], BF16)
    r_ktok = P.sb("r_ktok", [128, 256], BF16)
    r_sd = P.sb("r_sd", [128, 512], BF16)
    r_y = P.sb("r_y", [128, 2, TT], F32)
    r_ysq = P.sb("r_ysq", [128, TT], F32)
    r_mean = P.sb("r_mean", [128, TT], F32)
    r_var = P.sb("r_var", [128, TT], F32)

    dbg_tiles = {}

    def layer_tile_mixer(b, l, ti):
        lb = l * BPC + b
        r_S, r_Sb = r_S_l[l], r_Sb_l[l]
        t0 = ti * TT
        norm_mod(ti, lambda kc: gmodm[:, lb, kc:kc + 1], lambda kc: modT[:, lb, kc:kc + 1])
        if "ret" in mixers:
            for p in range(2):
                for (dst, nn, sn) in ((r_q, "rqn", "rqs"), (r_k, "rkn", "rks")):
                    psn = projB(l, "%s%d" % (nn, p), None)
                    pss = projB(l, "%s%d" % (sn, p), None)
                    P.tt(r_t1[:, :], psn[:, 0:TT], ropeb[ti % 2][:, 0, :], ALU.mult)
                    P.tt(r_t2[:, :], pss[:, 0:TT], ropeb[ti % 2][:, 1, :], ALU.mult)
                    P.tt(dst[:, p, :], r_t1[:, :], r_t2[:, :], ALU.add, eng="pool")
                P.tt(r_qd[:, p, :], r_q[:, p, :], C["ret_qd"][:, p, :], ALU.mult, eng="pool")
                psg = projB(l, "rg%d" % p, None)
                P.act(r_gs[:, p, :], psg[:, 0:TT], AF.Silu)
            if "r1" in mixers:
                P.memset(r_y[:, :, :], 1.0)
            for bi in range(TT // BL if "r1" not in mixers else 0):
                c0 = bi * BL
                psv = P.ps()
                for kc in range(8):
                    P.mm(psv[:, 0:256], hT[:, kc, c0:c0 + BL], wA[:, kc, 0:256], start=(kc == 0), stop=(kc == 7))
                P.cp(r_vtok[:, :], psv[:, 0:256], eng="act")
                if "r3" in mixers:
                    continue
                pk = P.pb()
                for p in range(2):
                    P.tr(pk[:, p * 128:(p + 1) * 128], r_k[:, p, c0:c0 + BL], C["ident_b"][:, :])
                P.tt(r_ktok[:, :], pk[:, 0:256], C["ret_kd"][:, :], ALU.mult)
                if "r4" in mixers:
                    continue
                pssA = P.ps()
                pssB = P.ps()
                for h in range(4):
                    p, hh = h // 2, h % 2
                    hp = 64 * hh
                    bank = pssA if hh == 0 else pssB
                    P.mm(bank[:, p * 128:(p + 1) * 128], r_k[hp:hp + 64, p, c0:c0 + BL], r_q[hp:hp + 64, p, c0:c0 + BL])
                for h in range(4):
                    p, hh = h // 2, h % 2
                    bank = pssA if hh == 0 else pssB
                    P.tt(r_sd[:, h * 128:(h + 1) * 128], bank[:, p * 128:(p + 1) * 128], C["ret_mdT"][:, h * 128:(h + 1) * 128], ALU.mult)
                if "r5" in mixers:
                    continue
                psy = P.ps()
                for h in range(4):
                    p, hp = h // 2, 64 * (h % 2)
                    o = psy[hp:hp + 64, p * 128:(p + 1) * 128]
                    P.mm(o, r_vtok[:, h * 64:(h + 1) * 64], r_sd[:, h * 128:(h + 1) * 128], start=True, stop=False)
                    P.mm(o, r_Sb[:, p * 128 + hp:p * 128 + hp + 64], r_qd[:, p, c0:c0 + BL], start=False, stop=True)
                for p in range(2):
                    P.cp(r_y[:, p, c0:c0 + BL], psy[:, p * 128:(p + 1) * 128], eng="act")
                psS = P.ps()
                for h in range(4):
                    p, hp = h // 2, 64 * (h % 2)
                    P.mm(psS[hp:hp + 64, p * 128 + hp:p * 128 + hp + 64], r_ktok[:, h * 64:(h + 1) * 64], r_vtok[:, h * 64:(h + 1) * 64])
                P.tt(r_S[:, :], r_S[:, :], C["ret_cd"][:, :], ALU.mult)
                for h in range(4):
                    p, hp = h // 2, 64 * (h % 2)
                    cs = slice(p * 128 + hp, p * 128 + hp + 64)
                    P.tt(r_S[hp:hp + 64, cs], r_S[hp:hp + 64, cs], psS[hp:hp + 64, cs], ALU.add)
                P.cp(r_Sb[:, :], r_S[:, :], eng="act")
            for p in range(2):
                head_norm_pair(r_y[:, p, :], PV[:, l, 64 + p:65 + p], None, r_gs[:, p, :], mixT[:, p, :])
        else:
            for p in range(2):
                P.memset(mixT[:, p, :], 0.0)
        if "rwkv" in mixers:
            rwkv_tile(b, l, ti)
        else:
            for p in range(2, 5):
                P.memset(mixT[:, p, :], 0.0)
        if "ml" in mixers:
            mlstm_tile(b, l, ti)
        else:
            for p in range(5, 8):
                P.memset(mixT[:, p, :], 0.0)
        for oc in range(8):
            wt = wbuf[wcnt[0] % 2]
            wcnt[0] += 1
            P.dma("sp", wt[:, :, :], SV(WOs[l, oc]))
            ps = P.ps()
            for kc in range(8):
                P.mm(ps[:, 0:TT], wt[:, kc, :], mixT[:, kc, :], start=(kc == 0), stop=(kc == 7))
            P.stt(xT[ti % 2][:, oc, :], ps[:, 0:TT], modT[:, lb, 16 + oc:17 + oc], xT[ti % 2][:, oc, :], ALU.mult, ALU.add)

    def head_norm_pair(y, gain_col, bonus, gate, out):
        psm = P.ps()
        P.mm(psm[:, 0:TT], C["blk64"][:, :], y, start=True, stop=True)
        P.act(r_ysq[:, :], y, AF.Square)
        psq = P.ps()
        P.mm(psq[:, 0:TT], C["blk64"][:, :], r_ysq[:, :], start=True, stop=True)
        P.cp(r_mean[:, :], psm[:, 0:TT], eng="act")
        P.tt(r_var[:, :], r_mean[:, :], r_mean[:, :], ALU.mult, eng="pool")
        P.tt(r_var[:, :], psq[:, 0:TT], r_var[:, :], ALU.subtract)
        rsqrt_eps(r_var[:, :], r_var[:, :], GN_EPS)
        P.tt(r_ysq[:, :], y, r_mean[:, :], ALU.subtract)
        P.stt(r_ysq[:, :], r_ysq[:, :], gain_col, r_var[:, :], ALU.mult, ALU.mult)
        if bonus is not None:
            P.tt(r_ysq[:, :], r_ysq[:, :], bonus, ALU.add, eng="pool")
        P.tt(out, r_ysq[:, :], gate, ALU.mult)

    m_raw = P.sb("m_raw", [128, 8, 3 + TT], F32)
    m_acc = P.sb("m_acc", [128, TT], F32)
    m_q = P.sb("m_q", [128, 4, TT], BF16)
    m_k = P.sb("m_k", [128, 4, TT], BF16)
    m_ip = P.sb("m_ip", [128, TT], F32)
    m_lf = P.sb("m_lf", [128, TT], F32)
    m_F = P.sb("m_F", [128, TT], F32)
    m_m = P.sb("m_m", [128, TT], F32)
    m_beta = P.sb("m_beta", [128, TT], F32)
    m_g = P.sb("m_g", [128, TT], F32)
    m_carry_l = [P.sb("m_carry%d" % i, [128, 2], F32) for i in range(L)]
    m_halo_l = [P.sb("m_halo%d" % i, [128, 8, 3], F32) for i in range(L)]
    m_bbh_l = [P.sb("m_bbh%d" % i, [128, 4, 2], F32) for i in range(L)]
    m_bb = P.sb("m_bb", [128, 4, 1 + TT], F32)
    m_nbb = P.sb("m_nbb", [128, 4, 1 + TT], F32)
    m_ngc = P.sb("m_ngc", [128, 4], F32)
    m_emm = P.sb("m_emm", [128, 4], F32)
    m_E = P.sb("m_E", [128, 512], F32)
    m_EM = P.sb("m_EM", [128, 512], F32)
    m_sD = P.sb("m_sD", [128, 512], BF16)
    m_int = P.sb("m_int", [128, 4, 128], F32)
    m_qi = P.sb("m_qi", [128, 4, 128], BF16)
    m_kws = P.sb("m_kws", [128, 4, 128], BF16)
    m_va = P.sb("m_va", [128, 4, 128], BF16)
    m_C_l = [P.sb("m_C%d" % i, [128, 4, 128], F32) for i in range(L)]
    m_Cb_l = [P.sb("m_Cb%d" % i, [128, 4, 128], BF16) for i in range(L)]
    m_dm = P.sb("m_dm", [128, 4], F32)
    m_hc = P.sb("m_hc", [128, 384], F32)
    m_hsq = m_EM[:, 0:384]
    m_st = P.sb("m_st", [128, 16], F32)
    m_so = P.sb("m_so", [128, 384], F32)
    m_mx = P.sb("m_mx", [128, 384], BF16)
    P.memset(m_kws[:, :, :], 0.0)
    for t_ in (m_ip, m_lf, m_F, m_m, m_beta, m_g):
        P.memset(t_[:, :], 0.0)

    def mlstm_tile(b, l, ti):
        m_carry, m_C, m_Cb = m_carry_l[l], m_C_l[l], m_Cb_l[l]
        P.cp(m_raw[:, :, 0:3], m_halo_l[l][:, :, :])
        P.cp(m_bb[:, :, 0:1], m_bbh_l[l][:, :, 0:1])
        P.cp(m_nbb[:, :, 0:1], m_bbh_l[l][:, :, 1:2])
        for b8 in range(8):
            nm = ("mq%d" % b8) if b8 < 4 else ("mk%d" % (b8 - 4))
            ps = projB(l, nm, None)
            P.cp(m_raw[:, b8, 3:3 + TT], ps[:, 0:TT], eng="act")
            pc = 96 + b8 * 5
            P.act(m_acc[:, :], ps[:, 0:TT], AF.Identity, bias=PV[:, l, pc + 4:pc + 5], scale=PV[:, l, pc + 3:pc + 4])
            for tap in range(3):
                P.stt(m_acc[:, :], m_raw[:, b8, tap:tap + TT], PV[:, l, pc + tap:pc + tap + 1], m_acc[:, :], ALU.mult, ALU.add)
            P.cp(m_halo_l[l][:, b8, :], m_raw[:, b8, TT:TT + 3])
            if b8 < 4:
                P.act(m_q[:, b8, :], m_acc[:, :], AF.Silu)
            else:
                P.act(m_k[:, b8 - 4, :], m_acc[:, :], AF.Silu)
        if "m0" in mixers:
            for j in range(3):
                P.memset(mixT[:, 5 + j, :], 0.0)
            return
        import os as _os
        mlv = int(_os.environ.get("MLV", "99"))

        def _bail():
            for j in range(3):
                P.memset(mixT[:, 5 + j, :], 0.0)
        psi = projB(l, "mi", None)
        P.act(m_ip[0:4, :], psi[0:4, 0:TT], AF.Identity, bias=PV[0:4, l, 140:141])
        if mlv <= 1:
            return _bail()
        psf_ = projB(l, "mf", None)
        P.act(m_lf[0:4, :], psf_[0:4, 0:TT], AF.Sigmoid, bias=PV[0:4, l, 141:142])
        if mlv <= 2:
            return _bail()
        P.act(m_lf[0:4, :], m_lf[0:4, :], AF.Ln)
        if mlv <= 3:
            return _bail()
        P.scan(m_F[0:4, :], C["ones_r"][0:4, :], m_lf[0:4, :], (0.0 if "m2" in mixers else m_carry[0:4, 0:1]), ALU.mult, ALU.add)
        P.scan(m_m[0:4, :], m_lf[0:4, :], m_ip[0:4, :], (0.0 if "m2" in mixers else m_carry[0:4, 1:2]), (ALU.mult if "m3" in mixers else ALU.add), (ALU.add if "m3" in mixers else ALU.max))
        if mlv <= 4:
            return _bail()
        P.tt(m_beta[0:4, :], m_F[0:4, :], m_m[0:4, :], ALU.subtract)
        P.tt(m_g[0:4, :], m_F[0:4, :], m_ip[0:4, :], ALU.subtract)
        P.cp(m_carry[0:4, 0:1], m_F[0:4, TT - 1:TT])
        P.cp(m_carry[0:4, 1:2], m_m[0:4, TT - 1:TT])
        if mlv <= 5:
            return _bail()
        for h in range(4):
            ps = P.ps()
            P.mm(ps[:, 0:TT], (C["blk1"][:, :] if mlv == 6 else C["sel4"][:, h, :]), m_beta[:, :], start=True, stop=True)
            if mlv == 7:
                continue
            P.cp(m_bb[:, h, 1:1 + TT], ps[:, 0:TT], eng="act")
            if mlv == 8:
                continue
            P.ts(m_nbb[:, h, 1:1 + TT], ps[:, 0:TT], -1.0, ALU.mult)
        if "m1" in mixers:
            for j in range(3):
                P.memset(mixT[:, 5 + j, :], 0.0)
        for bi in range(TT // BL if "m1" not in mixers else 0):
            c0 = bi * BL
            pst = P.ps()
            P.mm(pst[:, 0:4], m_g[:, c0:c0 + BL], C["ident_f"][:, 0:4])
            P.mm(pst[:, 4:8], m_m[:, c0:c0 + BL], C["ident_f"][:, 0:4])
            P.ts(m_ngc[:, :], pst[:, 0:4], -1.0, ALU.mult)
            P.act(m_emm[:, :], pst[:, 4:8], AF.Exp, scale=-1.0)
            psv = P.ps()
            for kc in range(8):
                P.mm(psv[:, 0:384], hT[:, kc, c0:c0 + BL], wA[:, kc, 256:640], start=(kc == 0), stop=(kc == 7))
            for h in range(4):
                P.cp(m_va[:, h, 0:96], psv[:, h * 96:(h + 1) * 96], eng="act")
            pso = P.ps()
            for kc in range(8):
                P.mm(pso[:, 0:384], hT[:, kc, c0:c0 + BL], wA[:, kc, 640:1024], start=(kc == 0), stop=(kc == 7))
            P.act(m_so[:, :], pso[:, 0:384], AF.Sigmoid)
            pss_ = P.ps()
            for h in range(4):
                P.mm(pss_[:, h * 128:(h + 1) * 128], m_k[:, h, c0:c0 + BL], m_q[:, h, c0:c0 + BL])
            for h in range(4):
                P.act(m_E[:, h * 128:(h + 1) * 128], m_bb[:, h, 1 + c0:1 + c0 + BL], AF.Exp, bias=m_ngc[:, h:h + 1])
                P.act(m_int[:, h, :], m_bb[:, h, 1 + c0:1 + c0 + BL], AF.Exp, bias=m_nbb[:, h, c0:c0 + 1])
            P.tt(m_EM[:, :], m_E[:, :], C["m_ml4"][:, :], ALU.mult)
            P.tt(m_sD[:, :], pss_[:, :], m_EM[:, :], ALU.mult)
            P.tt(m_qi[:, :, :], m_q[:, :, c0:c0 + BL], m_int[:, :, :], ALU.mult)
            pk = P.ps()
            for h in range(4):
                P.mm(pk[:, h * 96:(h + 1) * 96], m_k[:, h, c0:c0 + BL], C["ident_b"][:, 0:96])
            for h in range(4):
                P.ts(m_kws[:, h, 0:96], pk[:, h * 96:(h + 1) * 96], m_E[:, h * 128 + 127:h * 128 + 128], ALU.mult, 96.0 ** -0.5, ALU.mult)
            psn = P.ps()
            for h in range(4):
                P.mm(psn[:, h * 128:h * 128 + 97], m_sD[:, h * 128:(h + 1) * 128], m_va[:, h, 0:97], start=True, stop=False)
                P.mm(psn[:, h * 128:h * 128 + 97], m_qi[:, h, :], m_Cb[:, h, 0:97], start=False, stop=True)
            psC = P.ps()
            for h in range(4):
                P.mm(psC[:, h * 128:h * 128 + 97], m_kws[:, h, :], m_va[:, h, 0:97])
            for h in range(4):
                P.stt(m_C[:, h, 0:97], m_C[:, h, 0:97], m_int[:, h, 127:128], psC[:, h * 128:h * 128 + 97], ALU.mult, ALU.add)
            P.cp(m_Cb[:, :, :], m_C[:, :, :], eng="act")
            for h in range(4):
                P.cp(m_dm[:, h:h + 1], psn[:, h * 128 + 96:h * 128 + 97])
            P.ts(m_st[:, 12:16], m_dm[:, :], -1.0, ALU.mult)
            P.tt(m_dm[:, :], m_dm[:, :], m_st[:, 12:16], ALU.max)
            P.tt(m_dm[:, :], m_dm[:, :], m_emm[:, :], ALU.max)
            P.recip(m_dm[:, :], m_dm[:, :])
            for h in range(4):
                P.ts(m_hc[:, h * 96:(h + 1) * 96], psn[:, h * 128:h * 128 + 96], m_dm[:, h:h + 1], ALU.mult)
            P.red(m_st[:, 0:4], m_hc[:, :].rearrange("p (h e) -> p h e", h=4))
            P.tt(m_hsq, m_hc[:, :], m_hc[:, :], ALU.mult)
            P.red(m_st[:, 4:8], m_hsq.rearrange("p (h e) -> p h e", h=4))
            P.ts(m_st[:, 0:4], m_st[:, 0:4], 1.0 / 96.0, ALU.mult)
            P.tt(m_st[:, 8:12], m_st[:, 0:4], m_st[:, 0:4], ALU.mult)
            P.stt(m_st[:, 4:8], m_st[:, 4:8], 1.0 / 96.0, m_st[:, 8:12], ALU.mult, ALU.subtract)
            rsqrt_eps(m_st[:, 4:8], m_st[:, 4:8], GN_EPS)
            for h in range(4):
                P.ts(m_hc[:, h * 96:(h + 1) * 96], m_hc[:, h * 96:(h + 1) * 96], m_st[:, h:h + 1], ALU.subtract, m_st[:, 4 + h:5 + h], ALU.mult)
            P.tt(m_mx[:, :], m_hc[:, :], m_so[:, :], ALU.mult)
            pm = P.ps()
            for j in range(3):
                P.mm(pm[:, j * 128:(j + 1) * 128], m_mx[:, j * 128:(j + 1) * 128], C["ident_b"][:, :])
            for j in range(3):
                P.act(mixT[:, 5 + j, c0:c0 + BL], pm[:, j * 128:(j + 1) * 128], AF.Identity, scale=PV[:, l, 142 + j:143 + j])
        P.cp(m_bbh_l[l][:, :, 0:1], m_bb[:, :, TT:TT + 1])
        P.cp(m_bbh_l[l][:, :, 1:2], m_nbb[:, :, TT:TT + 1])

    w_d = P.sb("w_d", [128, TT], F32)
    w_tw = P.sb("w_tw", [128, TT], BF16)
    w_sg = P.sb("w_sg", [128, TT], BF16)
    w_a = P.sb("w_a", [128, 3, TT], F32)
    w_g = P.sb("w_g", [128, 3, TT], F32)
    w_ld = P.sb("w_ld", [128, 3, TT], F32)
    w_cum = P.sb("w_cum", [128, 3, TT], F32)
    w_kk = P.sb("w_kk", [128, 3, TT], F32)
    w_kp = P.sb("w_kp", [128, 3, TT], F32)
    w_bp = P.sb("w_bp", [128, 3, TT], F32)
    w_bon = P.sb("w_bon", [128, 3, TT], F32)
    w_e = P.sb("w_e", [128, TT], F32)
    w_rT = P.sb("w_rT", [128, 3, TT], BF16)
    w_aT = P.sb("w_aT", [128, 3, TT], BF16)
    w_bT = P.sb("w_bT", [128, 3, TT], BF16)
    w_kT = P.sb("w_kT", [128, 3, TT], BF16)
    w_bh = P.sb("w_bh", [128, 3, TT], BF16)
    w_kh = P.sb("w_kh", [128, 3, TT], BF16)
    w_vb = P.sb("w_vb", [128, 3, TT], BF16)
    w_WL = P.sb("w_WL", [128, 3, TT // BL], F32)
    w_S_l = [P.sb("w_S%d" % i, [128, 3, 128], F32) for i in range(L)]
    w_halo_l = [P.sb("w_halo%d" % i, [128, 11], F32) for i in range(L)]
    w_Sb_l = [P.sb("w_Sb%d" % i, [128, 3, 128], BF16) for i in range(L)]
    w_vtok = P.sb("w_vtok", [128, 384], BF16)
    w_bhtok = P.sb("w_bhtok", [128, 384], BF16)
    w_khtok = P.sb("w_khtok", [128, 384], BF16)
    w_AM = [P.sb("w_AM%d" % h, [128, 512], BF16) for h in range(6)]
    w_P = [P.sb("w_P%d" % i, [128, 384], F32) for i in range(4)]
    w_Q = [P.sb("w_Q%d" % i, [128, 384], F32) for i in range(4)]
    w_T = [P.sb("w_T%d" % i, [128, 384], F32) for i in range(2)]
    w_X = P.sb("w_X", [128, 384], F32)
    w_aTf = P.sb("w_aTf", [128, 3, TT], F32)
    w_bTf = P.sb("w_bTf", [128, 3, TT], F32)
    w_U = P.sb("w_U", [128, 384], BF16)
    w_y = P.sb("w_y", [128, 3, TT], F32)

    def rwkv_tile(b, l, ti):
        w_S, w_Sb = w_S_l[l], w_Sb_l[l]
        for j in range(11):
            P.cp(w_raw[:, j, 0:1], w_halo_l[l][:, j:j + 1])
        names = ["wr0", "wr1", "wr2", "wk0", "wk1", "wk2", "wv0", "wv1", "wv2", "wwa", "wgl"]
        for j, nm in enumerate(names):
            ps = projB(l, nm, None)
            P.cp(w_raw[:, j, 1:1 + TT], ps[:, 0:TT], eng="act")
            P.tt(w_d[:, :], w_raw[:, j, 0:TT], w_raw[:, j, 1:1 + TT], ALU.subtract)
            P.stt(w_sh[:, j, :], w_d[:, :], PV[:, l, 69 + j:70 + j], w_raw[:, j, 1:1 + TT], ALU.mult, ALU.add)
            P.cp(w_halo_l[l][:, j:j + 1], w_raw[:, j, TT:TT + 1])
        R = lambda p: w_sh[:, p, :]
        K = lambda p: w_sh[:, 3 + p, :]
        Vv = lambda p: w_sh[:, 6 + p, :]
        P.act(w_tw[0:64, :], w_sh[0:64, 9, :], AF.Tanh)
        P.cp(w_tw[64:128, :], w_sh[64:128, 9, :], eng="act")
        P.act(w_sg[:, :], w_sh[:, 10, :], AF.Sigmoid)
        for p in range(3):
            ps = P.ps()
            P.mm(ps[:, 0:TT], RWL[:, l, p * 128:(p + 1) * 128], w_tw[:, :])
            P.act(w_ld[:, p, :], ps[:, 0:TT], AF.Sigmoid, bias=PV[:, l, 80 + p:81 + p])
            P.ts(w_ld[:, p, :], w_ld[:, p, :], -math.exp(-0.5), ALU.mult)
            ps2 = P.ps()
            P.mm(ps2[:, 0:TT], RWL[:, l, 384 + p * 128:384 + (p + 1) * 128], w_tw[:, :])
            P.act(w_a[:, p, :], ps2[:, 0:TT], AF.Sigmoid, bias=PV[:, l, 83 + p:84 + p])
            ps3 = P.ps()
            P.mm(ps3[:, 0:TT], RWL[:, l, 768 + p * 128:768 + (p + 1) * 128], w_sg[:, :])
            P.cp(w_g[:, p, :], ps3[:, 0:TT], eng="act")
        for p in range(3):
            P.ts(w_kk[:, p, :], K(p), PV[:, l, 86 + p:87 + p], ALU.mult)
            P.tt(w_e[:, :], w_kk[:, p, :], w_kk[:, p, :], ALU.mult)
            ps = P.ps()
            P.mm(ps[:, 0:TT], C["blk1"][:, :], w_e[:, :])
            rsqrt_eps(w_e[:, :], ps[:, 0:TT], 1e-24, ALU.max)
            P.tt(w_kk[:, p, :], w_kk[:, p, :], w_e[:, :], ALU.mult)
            P.ts(w_e[:, :], w_a[:, p, :], PV[:, l, 89 + p:90 + p], ALU.mult, omka[:, l, p:p + 1], ALU.add)
            P.tt(w_kp[:, p, :], K(p), w_e[:, :], ALU.mult)
            P.stt(w_e[:, :], R(p), PV[:, l, 92 + p:93 + p], w_kp[:, p, :], ALU.mult, ALU.mult)
            ps = P.ps()
            P.mm(ps[:, 0:TT], C["blk1"][:, :], w_e[:, :])
            P.tt(w_bon[:, p, :], ps[:, 0:TT], Vv(p), ALU.mult)
            P.tt(w_bp[:, p, :], w_kk[:, p, :], w_a[:, p, :], ALU.mult)
            P.scan(w_cum[:, p, :], C["rst"][:, :], w_ld[:, p, :], 0.0, ALU.mult, ALU.add)
            P.act(w_e[:, :], w_cum[:, p, :], AF.Exp)
            P.tt(w_rT[:, p, :], R(p), w_e[:, :], ALU.mult)
            for bi in range(TT // BL):
                P.cp(w_WL[:, p, bi:bi + 1], w_e[:, bi * BL + BL - 1:bi * BL + BL])
            P.tt(w_e[:, :], w_cum[:, p, :], w_ld[:, p, :], ALU.subtract)
            P.act(w_e[:, :], w_e[:, :], AF.Exp)
            P.stt(w_aT[:, p, :], w_kk[:, p, :], -1.0, w_e[:, :], ALU.mult, ALU.mult)
            P.stt(w_aTf[:, p, :], w_kk[:, p, :], -1.0, w_e[:, :], ALU.mult, ALU.mult)
            P.act(w_e[:, :], w_cum[:, p, :], AF.Exp, scale=-1.0)
            P.tt(w_bT[:, p, :], w_bp[:, p, :], w_e[:, :], ALU.mult)
            P.tt(w_bTf[:, p, :], w_bp[:, p, :], w_e[:, :], ALU.mult)
            P.tt(w_kT[:, p, :], w_kp[:, p, :], w_e[:, :], ALU.mult)
            for bi in range(TT // BL):
                c0 = bi * BL
                P.act(w_e[:, c0:c0 + BL], w_cum[:, p, c0:c0 + BL], AF.Exp, bias=w_cum[:, p, c0 + BL - 1:c0 + BL], scale=-1.0)
            P.tt(w_bh[:, p, :], w_bp[:, p, :], w_e[:, :], ALU.mult)
            P.tt(w_kh[:, p, :], w_kp[:, p, :], w_e[:, :], ALU.mult)
            P.cp(w_vb[:, p, :], Vv(p), eng="act")
        for bi in range(TT // BL):
            c0 = bi * BL
            for (src, dst) in ((w_vb, w_vtok), (w_bh, w_bhtok), (w_kh, w_khtok)):
                pk = P.ps()
                for p in range(3):
                    P.mm(pk[:, p * 128:(p + 1) * 128], src[:, p, c0:c0 + BL], C["ident_b"][:, :])
                P.cp(dst[:, :], pk[:, 0:384], eng="act")
            for h in range(6):
                p, hp = h // 2, 64 * (h % 2)
                sl = slice(hp, hp + 64)
                ps = P.ps()
                P.mm(ps[:, 0:128], w_bTf[sl, p, c0:c0 + BL], w_aTf[sl, p, c0:c0 + BL])
                P.mm(ps[:, 128:256], w_bT[sl, p, c0:c0 + BL], w_rT[sl, p, c0:c0 + BL])
                P.mm(ps[:, 256:384], w_kT[sl, p, c0:c0 + BL], w_aT[sl, p, c0:c0 + BL])
                P.mm(ps[:, 384:512], w_kT[sl, p, c0:c0 + BL], w_rT[sl, p, c0:c0 + BL])
                P.tt(w_AM[h][:, 128:512], ps[:, 128:512], C["m_rw"][:, 128:512], ALU.mult)
                P.tt(w_P[2 * (h % 2)][:, p * 128:(p + 1) * 128], ps[:, 0:128], C["m_rw"][:, 0:128], ALU.mult)
            TTm = {}
            for g in range(2):
                sl = slice(64 * g, 64 * g + 64)
                psq = P.ps()
                for j in range(3):
                    P.mm(psq[:, j * 128:(j + 1) * 128], w_aTf[sl, j, c0:c0 + BL], w_bTf[sl, j, c0:c0 + BL])
                Pm, Qm, Tm = w_P[2 * g], w_Q[2 * g], w_T[g]
                for j in range(3):
                    P.tt(Qm[:, j * 128:(j + 1) * 128], psq[:, j * 128:(j + 1) * 128], C["m_lo"][:, :], ALU.mult)
                for j in range(3):
                    P.tt(Tm[:, j * 128:(j + 1) * 128], Pm[:, j * 128:(j + 1) * 128], C["ident_f"][:, :], ALU.add)
                cur = 0
                for lev in range(6):
                    Pn, Qn = w_P[2 * g + 1 - cur], w_Q[2 * g + 1 - cur]
                    Po, Qo = w_P[2 * g + cur], w_Q[2 * g + cur]
                    pp = P.ps()
                    pq = P.ps()
                    for j in range(3):
                        P.mm(pp[:, j * 128:(j + 1) * 128], Qo[:, j * 128:(j + 1) * 128], Po[:, j * 128:(j + 1) * 128])
                    for j in range(3):
                        P.mm(pq[:, j * 128:(j + 1) * 128], Po[:, j * 128:(j + 1) * 128], Qo[:, j * 128:(j + 1) * 128])
                    P.cp(Pn[:, :], pp[:, 0:384], eng="act")
                    P.cp(Qn[:, :], pq[:, 0:384])
                    pt = P.ps()
                    for j in range(3):
                        P.mm(pt[:, j * 128:(j + 1) * 128], Qn[:, j * 128:(j + 1) * 128], Tm[:, j * 128:(j + 1) * 128])
                    P.tt(Tm[:, :], Tm[:, :], pt[:, 0:384], ALU.add)
                    cur = 1 - cur
                TTm[g] = Tm
            psX = P.ps()
            for h in range(6):
                p, hp = h // 2, 64 * (h % 2)
                P.mm(psX[:, h * 64:(h + 1) * 64], w_aT[:, p, c0:c0 + BL], w_Sb[:, p, hp:hp + 64], start=True, stop=False)
                P.mm(psX[:, h * 64:(h + 1) * 64], w_AM[h][:, 256:384], w_vtok[:, h * 64:(h + 1) * 64], start=False, stop=True)
            P.cp(w_X[:, :], psX[:, 0:384], eng="act")
            psU = P.ps()
            for h in range(6):
                g, j = h % 2, h // 2
                P.mm(psU[:, h * 64:(h + 1) * 64], TTm[g][:, j * 128:(j + 1) * 128], w_X[:, h * 64:(h + 1) * 64])
            P.cp(w_U[:, :], psU[:, 0:384], eng="act")
            psY = P.ps()
            for h in range(6):
                p, hp = h // 2, 64 * (h % 2)
                o = psY[hp:hp + 64, p * 128:(p + 1) * 128]
                P.mm(o, w_Sb[:, p, hp:hp + 64], w_rT[:, p, c0:c0 + BL], start=True, stop=False)
                P.mm(o, w_U[:, h * 64:(h + 1) * 64], w_AM[h][:, 128:256], start=False, stop=False)
                P.mm(o, w_vtok[:, h * 64:(h + 1) * 64], w_AM[h][:, 384:512], start=False, stop=True)
            for p in range(3):
                P.cp(w_y[:, p, c0:c0 + BL], psY[:, p * 128:(p + 1) * 128], eng="act")
            psS = P.ps()
            for h in range(6):
                p, hp = h // 2, 64 * (h % 2)
                o = psS[hp:hp + 64, p * 128 + hp:p * 128 + hp + 64]
                P.mm(o, w_bhtok[:, h * 64:(h + 1) * 64], w_U[:, h * 64:(h + 1) * 64], start=True, stop=False)
                P.mm(o, w_khtok[:, h * 64:(h + 1) * 64], w_vtok[:, h * 64:(h + 1) * 64], start=False, stop=True)
            for h in range(6):
                p, hp = h // 2, 64 * (h % 2)
                P.stt(w_S[hp:hp + 64, p, hp:hp + 64], w_S[hp:hp + 64, p, hp:hp + 64], w_WL[hp:hp + 64, p, bi:bi + 1],
                      psS[hp:hp + 64, p * 128 + hp:p * 128 + hp + 64], ALU.mult, ALU.add)
            P.cp(w_Sb[:, :, :], w_S[:, :, :], eng="act")
        for p in range(3):
            head_norm_pair(w_y[:, p, :], PV[:, l, 66 + p:67 + p], w_bon[:, p, :], w_g[:, p, :], mixT[:, 2 + p, :])

    def layer_tile_ffn(b, l, ti):
        lb = l * BPC + b
        norm_mod(ti, lambda kc: gmodf[:, lb, kc:kc + 1], lambda kc: modT[:, lb, 24 + kc:25 + kc])
        for fb in range(NFB):
            wt = w1buf[fb % 2]
            P.dma("act" if fb % 2 else "sp", wt[:, :, :], SV(W1s[l, fb]))
            psg = P.ps()
            psu = P.ps()
            for kc in range(8):
                P.mm(psg[:, 0:TT], wt[:, kc, 0:128], hT[:, kc, :], start=(kc == 0), stop=(kc == 7))
            for kc in range(8):
                P.mm(psu[:, 0:TT], wt[:, kc, 128:256], hT[:, kc, :], start=(kc == 0), stop=(kc == 7))
            sg_ = sgt[fb % 2]
            P.act(sg_[:, :], psg[:, 0:TT], AF.Silu)
            P.tt(actT[:, fb, :], sg_[:, :], psu[:, 0:TT], ALU.mult)
        for oc in range(8):
            wt = w2buf[oc % 2]
            P.dma("act" if oc % 2 else "sp", wt[:, :, :], SV(W2s[l, oc]))
            ps = P.ps()
            for fb in range(NFB):
                P.mm(ps[:, 0:TT], wt[:, fb, :], actT[:, fb, :], start=(fb == 0), stop=(fb == NFB - 1))
            P.stt(xT[ti % 2][:, oc, :], ps[:, 0:TT], modT[:, lb, 40 + oc:41 + oc], xT[ti % 2][:, oc, :], ALU.mult, ALU.add)

    final_tokens = []
    gti = 0
    for b in range(BPC):
        for l in range(L):
            P.memset(r_S_l[l][:, :], 0.0)
            P.memset(r_Sb_l[l][:, :], 0.0)
            P.memset(m_halo_l[l][:, :, :], 0.0)
            P.memset(m_carry_l[l][:, :], 0.0)
            P.memset(m_bbh_l[l][:, :, :], 0.0)
            P.memset(m_C_l[l][:, :, :], 0.0)
            P.memset(m_Cb_l[l][:, :, :], 0.0)
            P.memset(w_halo_l[l][:, :], 0.0)
            P.memset(w_S_l[l][:, :, :], 0.0)
            P.memset(w_Sb_l[l][:, :, :], 0.0)
        P.memset(m_va[:, :, 96:97], 1.0)
        for ti in range(NT):
            x = xT[ti % 2]
            P.dma("sp", x[:, :, :], x_d[b, :, :, ti * TT:(ti + 1) * TT])
            P.dma("sp", ropeb[ti % 2][:, :, :], rope_d[:, :, ti * TT:(ti + 1) * TT])
            for l in range(L):
                P.dma("act", wA[:, :, :], SV(WAs[l]))
                if "nomix" not in mixers:
                    layer_tile_mixer(b, l, ti)
                if "noffn" not in mixers:
                    layer_tile_ffn(b, l, ti)
            psm = P.ps()
            for kc in range(8 if "nofinal" not in mixers else 0):
                sq_ = sq[kc % 2]
                P.act(sq_[:, :], x[:, kc, :], AF.Square)
                P.mm(psm[:, 0:TT], C["onesN"][:, :], sq_[:, :], start=(kc == 0), stop=(kc == 7))
            if "nofinal" not in mixers:
                rsqrt_eps(rstd[:, :], psm[:, 0:TT], RMS_EPS)
            for kc in range(8 if ("nofinal" not in mixers and "f3" not in mixers) else 0):
                P.stt(x[:, kc, :], x[:, kc, :], FN[:, kc:kc + 1], rstd[:, :], ALU.mult, ALU.mult)
            final_tokens.append(P.dma("sp", y_d[b, :, :, ti * TT:(ti + 1) * TT], x[:, :, :]))
    P.emit(final_tokens)
    es.close()
    return nc, consts, rope_np


_CACHE = {}


def run(inputs, n_cores, BPC, T, L, mixers=("ret", "rwkv", "ml"), lay=None):
    key = (BPC, T, L, tuple(mixers))
    if key not in _CACHE:
        _CACHE[key] = build_program(BPC, T, L, mixers)
    nc, consts, rope_np = _CACHE[key]
    if lay is None:
        lay = build_layer_inputs(inputs, L)
    x = np.asarray(inputs["x"], np.float32)
    c = np.asarray(inputs["c"], np.float32)
    in_maps = []
    for core in range(n_cores):
        xs = x[core * BPC:(core + 1) * BPC]
        xTm = np.ascontiguousarray(xs.reshape(BPC, T, 8, 128).transpose(0, 3, 2, 1))
        cs = c[core * BPC:(core + 1) * BPC]
        cTm = np.ascontiguousarray(cs.reshape(BPC, 8, 128).transpose(2, 1, 0))
        m = {"x": xTm, "cT": cTm, "rope": rope_np}
        m.update(lay)
        for k, v in consts.items():
            m["c_" + k] = v
        in_maps.append(m)
    res = run_bass_kernel_spmd(nc, in_maps, core_ids=list(range(n_cores)))
    outs = []
    for core in range(n_cores):
        yT = res.results[core]["y"]
        outs.append(np.ascontiguousarray(yT.transpose(0, 3, 2, 1)).reshape(BPC, T, 1024))
    return np.concatenate(outs, 0).astype(np.float32)


def kernel(**inputs):
    B, T, _ = inputs["x"].shape
    L = inputs["w_in"].shape[0]
    lay = build_layer_inputs(inputs, L)
    x = np.asarray(inputs["x"], np.float32)
    c = np.asarray(inputs["c"], np.float32)
    outs = []
    for r in range(B // 8):
        sub = {"x": x[r * 8:(r + 1) * 8], "c": c[r * 8:(r + 1) * 8]}
        outs.append(run(sub, 8, 1, T, L, lay=lay))
    return np.concatenate(outs, 0)
```

```python
import math
from contextlib import ExitStack
import numpy as np
import ml_dtypes
import concourse.bass as bass
import concourse.mybir as mybir
from concourse.bass_utils import run_bass_kernel_spmd

F32 = mybir.dt.float32
BF16 = mybir.dt.bfloat16
ALU = mybir.AluOpType
AF = mybir.ActivationFunctionType
AX = mybir.AxisListType

D = 1024
DFF = 2816
NFB = 22
RMS_EPS = 1e-6
GN_EPS = 1e-5
TT = 128
BL = 128


class Unit:
    __slots__ = ("name", "w", "r", "excl")

    def __init__(self, name):
        self.name = name
        self.w = None
        self.r = []
        self.excl = False


class V:
    __slots__ = ("unit", "ap")

    def __init__(self, unit, ap):
        self.unit = unit
        self.ap = ap

    def rearrange(self, pat, **kw):
        return V(self.unit, self.ap.rearrange(pat, **kw))

    def __getitem__(self, idx):
        return V(self.unit, self.ap[idx])


class TileH:
    def __init__(self, handle, name):
        self.h = handle
        self.unit = Unit(name)
        self.name = name

    def __getitem__(self, idx):
        return V(self.unit, self.h[idx])

    def sub(self, key):
        t = TileH.__new__(TileH)
        t.h = self.h
        t.unit = Unit(self.name + str(key))
        t.name = t.unit.name
        return t


ENGS = ("pe", "dve", "act", "pool", "sp")
INLINE_WAIT = True
NDS = 24


class Prog:
    def __init__(self, nc, es):
        self.nc = nc
        self.es = es
        self.ins = {e: [] for e in ENGS}
        self.ndma = 0
        self.dma_tokens = []
        self.psf = []
        self.psb = []
        self.ipf = 0
        self.ipb = 0
        self.epoch = 0
        self.last_real = {}

    def sb(self, name, shape, dt):
        h = self.es.enter_context(self.nc.sbuf_tensor("sb_" + name, list(shape), dt))
        return TileH(h, name)

    def alloc_psum(self):
        for i in range(8):
            h = self.es.enter_context(self.nc.psum_tensor("psf%d" % i, [128, 512], F32))
            self.psf.append(TileH(h, "psf%d" % i))
            self.psf[-1].unit.excl = True

    def ps(self):
        t = self.psf[self.ipf % 8]
        self.ipf += 1
        return t

    def pb(self):
        return self.ps()

    def _rec(self, eng, fn, reads, writes, dma=False, force=False):
        if eng == "pool" and not dma and not force:
            eng = "dve"
        deps = set()
        for v in reads:
            if isinstance(v, V) and v.unit.w is not None:
                deps.add(v.unit.w)
            if isinstance(v, V) and v.unit.excl:
                deps.update(v.unit.r)
        for v in writes:
            if isinstance(v, V):
                if v.unit.w is not None:
                    deps.add(v.unit.w)
                deps.update(v.unit.r)
        idx = len(self.ins[eng])
        if dma:
            did = self.ndma
            self.ndma += 1
            tok = ("dma", did)
            if did >= NDS:
                deps.add(("dma", did - NDS))
        else:
            did = None
            tok = (eng, idx)
        self.ins[eng].append({"fn": fn, "deps": deps, "sig": False, "dma": did, "ep": self.epoch})
        if not dma:
            self.last_real[eng] = idx
        for v in reads:
            if isinstance(v, V):
                v.unit.r.append(tok)
        for v in writes:
            if isinstance(v, V):
                v.unit.w = tok
                v.unit.r = []
        return tok

    @staticmethod
    def _ap(v):
        return v.ap if isinstance(v, V) else v

    def mm(self, out, lhsT, rhs, start=True, stop=True):
        o, l, r = self._ap(out), self._ap(lhsT), self._ap(rhs)
        self._rec("pe", lambda e: e.matmul(o, l, r, start=start, stop=stop), [lhsT, rhs], [out])

    def tr(self, out, in_, ident):
        self.mm(out, in_, ident)

    def act(self, out, in_, func, bias=0.0, scale=1.0, eng="act"):
        o, i = self._ap(out), self._ap(in_)
        b, s = self._ap(bias), self._ap(scale)
        rd = [in_] + [x for x in (bias, scale) if isinstance(x, V)]
        self._rec("act", lambda e: e.activation(out=o, in_=i, func=func, bias=b, scale=s), rd, [out])

    def tt(self, out, in0, in1, op, eng="dve"):
        o, a, b = self._ap(out), self._ap(in0), self._ap(in1)
        self._rec(eng, lambda e: e.tensor_tensor(out=o, in0=a, in1=b, op=op), [in0, in1], [out])

    def ts(self, out, in0, s1, op0, s2=None, op1=None, eng="dve"):
        o, a = self._ap(out), self._ap(in0)
        x1, x2 = self._ap(s1), self._ap(s2)
        rd = [in0] + [x for x in (s1, s2) if isinstance(x, V)]
        if op1 is None:
            self._rec(eng, lambda e: e.tensor_scalar(out=o, in0=a, scalar1=x1, scalar2=None, op0=op0), rd, [out])
        else:
            self._rec(eng, lambda e: e.tensor_scalar(out=o, in0=a, scalar1=x1, scalar2=x2, op0=op0, op1=op1), rd, [out])

    def stt(self, out, in0, scalar, in1, op0, op1, eng="dve"):
        o, a, b, s = self._ap(out), self._ap(in0), self._ap(in1), self._ap(scalar)
        rd = [in0, in1] + ([scalar] if isinstance(scalar, V) else [])
        self._rec(eng, lambda e: e.scalar_tensor_tensor(out=o, in0=a, scalar=s, in1=b, op0=op0, op1=op1), rd, [out])

    def cp(self, out, in_, eng="dve"):
        o, i = self._ap(out), self._ap(in_)
        if eng == "act":
            self._rec("act", lambda e: e.activation(out=o, in_=i, func=AF.Copy), [in_], [out])
        else:
            self._rec(eng, lambda e: e.tensor_copy(out=o, in_=i), [in_], [out])

    def memset(self, out, val, eng="dve", force=False):
        o = self._ap(out)
        self._rec(eng, lambda e: e.memset(o, val), [], [out], force=force)

    def scan(self, out, d0, d1, init, op0, op1):
        o, a, b, i = self._ap(out), self._ap(d0), self._ap(d1), self._ap(init)
        rd = [d0, d1] + ([init] if isinstance(init, V) else [])
        self._rec("dve", lambda e: e.tensor_tensor_scan(out=o, data0=a, data1=b, initial=i, op0=op0, op1=op1), rd, [out])

    def red(self, out, in_, op=ALU.add):
        o, i = self._ap(out), self._ap(in_)
        self._rec("dve", lambda e: e.tensor_reduce(out=o, in_=i, axis=AX.X, op=op), [in_], [out])

    def recip(self, out, in_):
        o, i = self._ap(out), self._ap(in_)
        self._rec("dve", lambda e: e.reciprocal(out=o, in_=i), [in_], [out])

    def dma(self, q, out, in_):
        o, i = self._ap(out), self._ap(in_)
        tok = self._rec(q, lambda e: e.dma_start(out=o, in_=i), [in_], [out], dma=True)
        self.dma_tokens.append(tok)
        return tok

    def barrier(self):
        deps = set(("dma", d) for d in range(max(0, self.ndma - NDS), self.ndma))
        for e, idx in self.last_real.items():
            deps.add((e, idx))
        for e in ENGS:
            self.ins[e].append({"fn": None, "deps": set(deps), "sig": False, "dma": None, "ep": self.epoch})
        self.epoch += 1
        self.last_real = {}

    def emit(self, final_tokens):
        nc = self.nc
        es = self.es
        nep = self.epoch + 1
        sems = {(k, e): es.enter_context(nc.semaphore("s%d_%s" % (k, e))) for k in range(nep) for e in ENGS}
        dsem = [es.enter_context(nc.semaphore("d%d" % i)) for i in range(NDS)]
        self.ins["sp"].append({"fn": None, "deps": set(final_tokens), "sig": False, "dma": None, "ep": self.epoch})
        for e in ENGS:
            for idx, rec in enumerate(self.ins[e]):
                for tok in rec["deps"]:
                    if tok[0] == "dma":
                        continue
                    pe, pidx = tok
                    if pe == e and (e == "pe" or idx - pidx > 3):
                        continue
                    if self.ins[pe][pidx]["ep"] < rec["ep"]:
                        continue
                    self.ins[pe][pidx]["sig"] = True
        cnt = {}
        for e in ENGS:
            c = 0
            ep = 0
            arr = []
            for rec in self.ins[e]:
                if rec["ep"] != ep:
                    ep = rec["ep"]
                    c = 0
                if rec["sig"]:
                    c += 1
                arr.append(c)
            cnt[e] = arr
        block = es.enter_context(nc.Block())

        def emit_engine(e, eng):
            seen = {x: 0 for x in ENGS}
            seend = [0] * NDS
            cur_ep = 0
            for idx, rec in enumerate(self.ins[e]):
                if rec["ep"] != cur_ep:
                    cur_ep = rec["ep"]
                    seen = {x: 0 for x in ENGS}
                need = {}
                needd = {}
                for tok in rec["deps"]:
                    if tok[0] == "dma":
                        did = tok[1]
                        slot = did % NDS
                        val = 16 * (did // NDS + 1)
                        if val > seend[slot]:
                            needd[slot] = max(needd.get(slot, 0), val)
                    else:
                        pe, pidx = tok
                        if pe == e and (e == "pe" or idx - pidx > 3):
                            continue
                        if self.ins[pe][pidx]["ep"] < rec["ep"]:
                            continue
                        val = cnt[pe][pidx]
                        if val > seen[pe]:
                            need[pe] = max(need.get(pe, 0), val)
                waits = []
                for pe, val in need.items():
                    waits.append((sems[(rec["ep"], pe)], val))
                    seen[pe] = val
                for slot, val in needd.items():
                    waits.append((dsem[slot], val))
                    seend[slot] = val
                inline = None
                if rec["fn"] is not None and waits and INLINE_WAIT:
                    inline = waits.pop()
                for sm, val in waits:
                    eng.wait_ge(sm, val)
                if rec["fn"] is None:
                    continue
                inst = rec["fn"](eng)
                if inline is not None:
                    inst._wait_ge(inline[0], inline[1])
                if rec["dma"] is not None:
                    inst.then_inc(dsem[rec["dma"] % NDS], 16)
                elif rec["sig"]:
                    inst.then_inc(sems[(rec["ep"], e)], 1)

        block.tensor(lambda eng: emit_engine("pe", eng))
        block.vector(lambda eng: emit_engine("dve", eng))
        block.scalar(lambda eng: emit_engine("act", eng))
        block.gpsimd(lambda eng: emit_engine("pool", eng))
        block.sync(lambda eng: emit_engine("sp", eng))


RET_OFF = 0
RWKV_OFF = 1024
ML_OFF = 1024 + 1408


def _swap_half(cols):
    out = []
    for h in range(0, len(cols), 64):
        blk = cols[h:h + 64]
        out += blk[32:] + blk[:32]
    return out


def colblocks_B():
    blocks = []
    rq = list(range(0, 256))
    rk = list(range(256, 512))
    rg = list(range(768, 1024))
    for p in range(2):
        blocks.append(("rqn%d" % p, rq[128 * p:128 * p + 128]))
    for p in range(2):
        blocks.append(("rqs%d" % p, _swap_half(rq)[128 * p:128 * p + 128]))
    for p in range(2):
        blocks.append(("rkn%d" % p, rk[128 * p:128 * p + 128]))
    for p in range(2):
        blocks.append(("rks%d" % p, _swap_half(rk)[128 * p:128 * p + 128]))
    for p in range(2):
        blocks.append(("rg%d" % p, rg[128 * p:128 * p + 128]))
    o = RWKV_OFF
    for nm, base in (("wr", 0), ("wk", 384), ("wv", 768)):
        for p in range(3):
            blocks.append(("%s%d" % (nm, p), list(range(o + base + 128 * p, o + base + 128 * p + 128))))
    blocks.append(("wwa", list(range(o + 1152, o + 1152 + 128))))
    blocks.append(("wgl", list(range(o + 1280, o + 1408))))
    o = ML_OFF
    for h in range(4):
        blocks.append(("mq%d" % h, list(range(o + 96 * h, o + 96 * h + 96))))
    for h in range(4):
        blocks.append(("mk%d" % h, list(range(o + 384 + 96 * h, o + 384 + 96 * h + 96))))
    blocks.append(("mi", list(range(o + 1536, o + 1540))))
    blocks.append(("mf", list(range(o + 1540, o + 1544))))
    return blocks


COLS_A = list(range(512, 768)) + list(range(ML_OFF + 768, ML_OFF + 1152)) + list(range(ML_OFF + 1152, ML_OFF + 1536))


def _kmajor(w):
    C = w.shape[1]
    return np.ascontiguousarray(w.reshape(8, 128, C).transpose(1, 0, 2))


def pcol(v, n):
    return np.ascontiguousarray(np.asarray(v, np.float32).reshape(n, 128).T)


def build_consts(T):
    c = {}
    c["ident_f"] = np.eye(128, dtype=np.float32)
    c["ident_b"] = np.eye(128, dtype=np.float32).astype(ml_dtypes.bfloat16)
    c["onesN"] = np.full((128, 128), 1.0 / 1024.0, np.float32).astype(ml_dtypes.bfloat16)
    blk = np.zeros((128, 128), np.float32)
    blk[:64, :64] = 1.0
    blk[64:, 64:] = 1.0
    c["blk1"] = blk
    c["blk64"] = blk / 64.0
    inv = 10000.0 ** (-np.arange(0, 64, 2, dtype=np.float64) / 64.0)
    t = np.arange(T, dtype=np.float64)
    ang = t[None, :] * inv[:, None]
    cos = np.cos(ang)
    sin = np.sin(ang)
    cosT = np.concatenate([cos, cos, cos, cos], 0)
    sinT = np.concatenate([-sin, sin, -sin, sin], 0)
    rope = np.ascontiguousarray(np.stack([cosT, sinT], 1)).astype(np.float32)
    lg = np.log1p(-np.exp2(-5.0 - np.arange(4, dtype=np.float64)))
    idx = np.arange(BL, dtype=np.float64)
    diff = idx[None, :] - idx[:, None]
    md = np.zeros((128, 4, 128), np.float64)
    for h in range(4):
        md[:, h, :] = np.where(diff >= 0, np.exp(lg[h] * np.maximum(diff, 0)), 0.0) * 0.125
    c["ret_mdT"] = md.astype(np.float32).reshape(128, 512)
    qd = np.zeros((128, 2, TT), np.float64)
    for p in range(2):
        for hh in range(2):
            h = 2 * p + hh
            row = np.exp(lg[h] * (idx + 1.0))
            qd[64 * hh:64 * hh + 64, p, :] = np.tile(row, TT // BL)[None, :]
    c["ret_qd"] = qd.astype(np.float32)
    kd = np.zeros((128, 4, 64), np.float64)
    for h in range(4):
        kd[:, h, :] = np.exp(lg[h] * (BL - 1.0 - idx))[:, None] * 0.125
    c["ret_kd"] = kd.astype(np.float32).reshape(128, 256)
    cd = np.zeros((128, 2, 128), np.float64)
    for p in range(2):
        for hh in range(2):
            cd[64 * hh:64 * hh + 64, p, :] = np.exp(lg[2 * p + hh] * BL)
    c["ret_cd"] = cd.astype(np.float32).reshape(128, 256)
    up_inc = (idx[:, None] <= idx[None, :]).astype(np.float32)
    up_str = (idx[:, None] < idx[None, :]).astype(np.float32)
    lo_str = (idx[:, None] > idx[None, :]).astype(np.float32)
    c["m_ml4"] = (np.ascontiguousarray(np.broadcast_to(up_inc[:, None, :], (128, 4, 128))) * (96.0 ** -0.5)).astype(np.float32).reshape(128, 512)
    c["m_rw"] = np.ascontiguousarray(np.stack([up_str, up_inc, up_str, up_inc], 1)).astype(np.float32).reshape(128, 512)
    c["m_lo"] = lo_str
    rst = np.ones((128, TT), np.float32)
    rst[:, ::BL] = 0.0
    c["rst"] = rst
    c["ones_r"] = np.ones((128, TT), np.float32)
    ep = np.zeros((128, 2), np.float32)
    ep[:, 0] = RMS_EPS
    ep[:, 1] = GN_EPS
    c["epsc"] = ep
    sel = np.zeros((128, 4, 128), np.float32)
    for h in range(4):
        sel[h, h, :] = 1.0
    c["sel4"] = sel
    return c, rope


def build_layer_inputs(inp, L):
    out = {}
    blocks = colblocks_B()
    NB = len(blocks)
    w_in = np.asarray(inp["w_in"], np.float32)
    WB = np.zeros((L, NB, 128, 8, 128), np.float32)
    for l in range(L):
        for b, (nm, cols) in enumerate(blocks):
            WB[l, b, :, :, :len(cols)] = _kmajor(w_in[l][:, cols])
    out["WB"] = WB
    WA = np.zeros((L, 128, 8, 1024), np.float32)
    for l in range(L):
        WA[l] = _kmajor(w_in[l][:, COLS_A])
    out["WA"] = WA
    w_out = np.asarray(inp["w_out"], np.float32)
    WO = np.zeros((L, 8, 128, 8, 128), np.float32)
    for l in range(L):
        km = _kmajor(w_out[l])
        for oc in range(8):
            WO[l, oc] = km[:, :, oc * 128:(oc + 1) * 128]
    out["WO"] = WO
    f1 = np.asarray(inp["ffn_w_in"], np.float32)
    W1 = np.zeros((L, NFB, 128, 8, 256), np.float32)
    for l in range(L):
        km = _kmajor(f1[l])
        for fb in range(NFB):
            W1[l, fb, :, :, 0:128] = km[:, :, fb * 128:(fb + 1) * 128]
            W1[l, fb, :, :, 128:256] = km[:, :, DFF + fb * 128:DFF + (fb + 1) * 128]
    out["W1"] = W1
    f2 = np.asarray(inp["ffn_w_out"], np.float32)
    W2 = np.zeros((L, 8, 128, NFB, 128), np.float32)
    for l in range(L):
        km = np.ascontiguousarray(f2[l].reshape(NFB, 128, 1024).transpose(1, 0, 2))
        for oc in range(8):
            W2[l, oc] = km[:, :, oc * 128:(oc + 1) * 128]
    out["W2"] = W2
    ada = np.asarray(inp["ada_w"], np.float32)
    ADA = np.zeros((L, 48, 128, 8, 128), np.float32)
    for l in range(L):
        km = _kmajor(ada[l])
        for cb in range(48):
            ADA[l, cb] = km[:, :, cb * 128:(cb + 1) * 128]
    out["ADA"] = ADA
    pv = np.zeros((L, 128, 192), np.float32)
    rwl = np.zeros((L, 128, 1152), np.float32)
    for l in range(L):
        j = 0
        pv[l, :, 0:48] = pcol(inp["ada_b"][l], 48)
        pv[l, :, 48:56] = pcol(inp["norm_mix"][l], 8)
        pv[l, :, 56:64] = pcol(inp["norm_ffn"][l], 8)
        gn = np.asarray(inp["mix_gn"][l], np.float32)
        pv[l, :, 64:66] = pcol(gn[0:256], 2)
        pv[l, :, 66:69] = pcol(gn[256:640], 3)
        mu = np.asarray(inp["rwkv_mu"][l], np.float32)
        pv[l, :, 69:80] = pcol(mu, 11)
        pv[l, :, 80:83] = pcol(inp["rwkv_w0"][l], 3)
        pv[l, :, 83:86] = pcol(inp["rwkv_a0"][l], 3)
        pv[l, :, 86:89] = pcol(inp["rwkv_k_k"][l], 3)
        pv[l, :, 89:92] = pcol(inp["rwkv_k_a"][l], 3)
        pv[l, :, 92:95] = pcol(np.asarray(inp["rwkv_r_k"][l], np.float32).reshape(-1), 3)
        cw = np.asarray(inp["mlstm_conv_w"][l], np.float32)
        cb_ = np.asarray(inp["mlstm_conv_b"][l], np.float32)
        for b8 in range(8):
            ch = slice(96 * b8, 96 * b8 + 96)
            for tap in range(4):
                pv[l, 0:96, 96 + b8 * 5 + tap] = cw[tap, ch]
            pv[l, 0:96, 96 + b8 * 5 + 4] = cb_[ch]
        pv[l, 0:4, 140] = np.asarray(inp["mlstm_i_b"][l], np.float32)
        pv[l, 0:4, 141] = np.asarray(inp["mlstm_f_b"][l], np.float32)
        rwl[l, 0:64, 0:384] = inp["rwkv_w2"][l]
        rwl[l, 64:128, 384:768] = inp["rwkv_a2"][l]
        rwl[l, :, 768:1152] = inp["rwkv_g2"][l]
        pv[l, :, 142:145] = pcol(gn[640:1024], 3)
    out["PV"] = pv
    out["RWL"] = rwl
    out["FNORM"] = pcol(inp["final_norm"], 8)
    return out


def build_program(BPC, T, L, mixers=("ret", "rwkv", "ml"), dbg=()):
    NT = T // TT
    blocks = colblocks_B()
    NB = len(blocks)
    bidx = {nm: i for i, (nm, _) in enumerate(blocks)}
    bM = {nm: 128 for nm, cols in blocks}
    nc = bass.Bass("TRN2", target_bir_lowering=False)
    es = ExitStack()
    P = Prog(nc, es)

    def din(name, shape, dt=F32):
        return nc.dram_tensor(name, list(shape), dt, kind="ExternalInput").ap()

    x_d = din("x", [BPC, 128, 8, T])
    c_d = din("cT", [128, 8, BPC])
    WB_d = din("WB", [L, NB, 128, 8, 128])
    WA_d = din("WA", [L, 128, 8, 1024])
    WO_d = din("WO", [L, 8, 128, 8, 128])
    W1_d = din("W1", [L, NFB, 128, 8, 256])
    W2_d = din("W2", [L, 8, 128, NFB, 128])
    ADA_d = din("ADA", [L, 48, 128, 8, 128])
    PV_d = din("PV", [L, 128, 192])
    RWL_d = din("RWL", [L, 128, 1152])
    FN_d = din("FNORM", [128, 8])
    consts, rope_np = build_consts(T)
    rope_d = din("rope", [128, 2, T])
    cdram = {}
    for k, v in consts.items():
        cdram[k] = din("c_" + k, v.shape, BF16 if v.dtype == ml_dtypes.bfloat16 else F32)
    y_d = nc.dram_tensor("y", [BPC, 128, 8, T], F32, kind="ExternalOutput").ap()
    dbg_d = {}
    WBs = nc.dram_tensor("WBs", [L, NB, 128, 8, 128], BF16, kind="Internal").ap()
    WAs = nc.dram_tensor("WAs", [L, 128, 8, 1024], BF16, kind="Internal").ap()
    WOs = nc.dram_tensor("WOs", [L, 8, 128, 8, 128], BF16, kind="Internal").ap()
    W1s = nc.dram_tensor("W1s", [L, NFB, 128, 8, 256], BF16, kind="Internal").ap()
    W2s = nc.dram_tensor("W2s", [L, 8, 128, NFB, 128], BF16, kind="Internal").ap()

    P.alloc_psum()
    C = {}
    for k, v in consts.items():
        shp = list(v.shape)
        if True:
            C[k] = P.sb("k_" + k, shp, BF16 if v.dtype == ml_dtypes.bfloat16 else F32)
        P.dma("sp", C[k][:], cdram[k])
    cast_tokens = []
    for l in range(L if "nocast" not in mixers else 0):
        for b in range(NB):
            cast_tokens.append(P.dma("pool", WBs[l, b], WB_d[l, b]))
        for kc in range(8):
            cast_tokens.append(P.dma("pool", WAs[l, :, kc, :], WA_d[l, :, kc, :]))
        for oc in range(8):
            cast_tokens.append(P.dma("pool", WOs[l, oc], WO_d[l, oc]))
            cast_tokens.append(P.dma("pool", W2s[l, oc], W2_d[l, oc]))
        for fb in range(NFB):
            cast_tokens.append(P.dma("pool", W1s[l, fb], W1_d[l, fb]))
    scratch_unit = Unit("scratch")
    P.ins["pool"].append({"fn": None, "deps": set(cast_tokens), "sig": False, "dma": None, "ep": 0})
    barrier_tok = ("pool", len(P.ins["pool"]) - 1)
    kb = P.sb("kbar", [128, 8], F32)
    P.memset(kb[:, :], 0.0, eng="pool", force=True)
    scratch_unit.w = ("pool", len(P.ins["pool"]) - 1)

    def SV(ap):
        return V(scratch_unit, ap)

    PV = P.sb("PV", [128, L, 192], F32)
    w_raw = P.sb("w_raw", [128, 11, 1 + TT], F32)
    w_sh = P.sb("w_sh", [128, 11, TT], F32)
    RWLf = w_sh[:, 0:9, :]
    RWL = P.sb("RWL", [128, L, 1152], BF16)
    FN = P.sb("FN", [128, 8], F32)
    for l in range(L):
        P.dma("sp", PV[:, l, :], PV_d[l])
        P.dma("sp", RWLf, RWL_d[l].rearrange("p (a b) -> p a b", b=128))
        P.cp(RWL[:, l, :].rearrange("p (a b) -> p a b", b=128), RWLf)
    P.dma("sp", FN[:, :], FN_d)
    omka = P.sb("omka", [128, L, 3], F32)
    for l in range(L):
        P.ts(omka[:, l, :], PV[:, l, 89:92], -1.0, ALU.mult, 1.0, ALU.add)

    cT = P.sb("cT", [128, 8, BPC], F32)
    P.dma("sp", cT[:, :, :], c_d)
    cond = P.sb("cond", [128, 8, BPC], F32)
    P.act(cond[:, :, :], cT[:, :, :], AF.Silu)
    modT = P.sb("modT", [128, L * BPC, 48], F32)
    gmodm = P.sb("gmodm", [128, L * BPC, 8], F32)
    gmodf = P.sb("gmodf", [128, L * BPC, 8], F32)
    adaw = [w_sh[:, 0:8, :], w_raw[:, 0:8, 0:TT]]
    for l in range(L if "noada" not in mixers else 0):
        for cb in range(48):
            wt = adaw[(l * 48 + cb) % 2]
            P.dma("sp", wt[:, :, :], ADA_d[l, cb])
            ps = P.ps()
            for kc in range(8):
                P.mm(ps[:, 0:BPC], wt[:, kc, :], cond[:, kc, :], start=(kc == 0), stop=(kc == 7))
            for b in range(BPC):
                P.act(modT[:, l * BPC + b, cb:cb + 1], ps[:, b:b + 1], AF.Identity, bias=PV[:, l, cb:cb + 1])
        for b in range(BPC):
            lb = l * BPC + b
            P.ts(gmodm[:, lb, :], modT[:, lb, 8:16], 1.0, ALU.add)
            P.tt(gmodm[:, lb, :], gmodm[:, lb, :], PV[:, l, 48:56], ALU.mult)
            P.ts(gmodf[:, lb, :], modT[:, lb, 32:40], 1.0, ALU.add)
            P.tt(gmodf[:, lb, :], gmodf[:, lb, :], PV[:, l, 56:64], ALU.mult)

    xT = [P.sb("xT%d" % i, [128, 8, TT], F32) for i in range(2)]
    ropeb = [P.sb("rope%d" % i, [128, 2, TT], F32) for i in range(1)] * 2
    hT = P.sb("hT", [128, 8, TT], BF16)
    sq = [P.sb("sq%d" % i, [128, TT], BF16) for i in range(2)]
    rstd = P.sb("rstd", [128, TT], F32)
    tmpn = [P.sb("tmpn%d" % i, [128, TT], F32) for i in range(1)] * 2
    wbuf = [P.sb("wbuf%d" % i, [128, 8, 128], BF16) for i in range(2)]
    wA = P.sb("wA", [128, 8, 1024], BF16)
    mixT = P.sb("mixT", [128, 8, TT], BF16)
    w1buf = [P.sb("w1b%d" % i, [128, 8, 256], BF16) for i in range(1)] * 2
    w2buf = [P.sb("w2b%d" % i, [128, NFB, 128], BF16) for i in range(1)] * 2
    actT = P.sb("actT", [128, NFB, TT], BF16)
    sgt = [P.sb("sgt%d" % i, [128, TT], F32) for i in range(1)] * 2
    wcnt = [0]

    def load_wblock(l, nm):
        wt = wbuf[wcnt[0] % 2]
        wcnt[0] += 1
        P.dma("sp", wt[:, :, :], SV(WBs[l, bidx[nm]]))
        return wt

    def projB(l, nm, ps_view_fn):
        wt = load_wblock(l, nm)
        M = bM[nm]
        ps = P.ps()
        for kc in range(8):
            P.mm(ps[0:M, 0:TT], wt[:, kc, 0:M], hT[:, kc, :], start=(kc == 0), stop=(kc == 7))
        return ps

    def rsqrt_eps(dst, src, eps, op=ALU.add):
        P.ts(dst, src, eps, op)
        P.act(dst, dst, AF.Ln)
        P.act(dst, dst, AF.Exp, scale=-0.5)

    def norm_mod(ti, gm_col, sh_col):
        x = xT[ti % 2]
        psm = P.ps()
        for kc in range(8):
            s = sq[kc % 2]
            P.act(s[:, :], x[:, kc, :], AF.Square)
            P.mm(psm[:, 0:TT], C["onesN"][:, :], s[:, :], start=(kc == 0), stop=(kc == 7))
        rsqrt_eps(rstd[:, :], psm[:, 0:TT], RMS_EPS)
        for kc in range(8):
            tm = tmpn[kc % 2]
            P.tt(tm[:, :], x[:, kc, :], rstd[:, :], ALU.mult)
            P.act(hT[:, kc, :], tm[:, :], AF.Identity, bias=sh_col(kc), scale=gm_col(kc))

    r_q = P.sb("r_q", [128, 2, TT], BF16)
    r_qd = P.sb("r_qd", [128, 2, TT], BF16)
    r_k = P.sb("r_k", [128, 2, TT], BF16)
    r_t1 = P.sb("r_t1", [128, TT], F32)
    r_t2 = P.sb("r_t2", [128, TT], F32)
    r_gs = P.sb("r_gs", [128, 2, TT], F32)
    r_S_l = [P.sb("r_S%d" % i, [128, 256], F32) for i in range(L)]
    r_Sb_l = [P.sb("r_Sb%d" % i, [128, 256], BF16) for i in range(L)]
    r_vtok = P.sb("r_vtok", [128, 256], BF16)
    r_ktok = P.sb("r_ktok", [128, 256], BF16)
    r_sd = P.sb("r_sd", [128, 512], BF16)
    r_y = P.sb("r_y", [128, 2, TT], F32)
    r_ysq = P.sb("r_ysq", [128, TT], F32)
    r_mean = P.sb("r_mean", [128, TT], F32)
    r_var = P.sb("r_var", [128, TT], F32)

    dbg_tiles = {}

    def layer_tile_mixer(b, l, ti):
        lb = l * BPC + b
        r_S, r_Sb = r_S_l[l], r_Sb_l[l]
        t0 = ti * TT
        norm_mod(ti, lambda kc: gmodm[:, lb, kc:kc + 1], lambda kc: modT[:, lb, kc:kc + 1])
        if "ret" in mixers:
            for p in range(2):
                for (dst, nn, sn) in ((r_q, "rqn", "rqs"), (r_k, "rkn", "rks")):
                    psn = projB(l, "%s%d" % (nn, p), None)
                    pss = projB(l, "%s%d" % (sn, p), None)
                    P.tt(r_t1[:, :], psn[:, 0:TT], ropeb[ti % 2][:, 0, :], ALU.mult)
                    P.tt(r_t2[:, :], pss[:, 0:TT], ropeb[ti % 2][:, 1, :], ALU.mult)
                    P.tt(dst[:, p, :], r_t1[:, :], r_t2[:, :], ALU.add, eng="pool")
                P.tt(r_qd[:, p, :], r_q[:, p, :], C["ret_qd"][:, p, :], ALU.mult, eng="pool")
                psg = projB(l, "rg%d" % p, None)
                P.act(r_gs[:, p, :], psg[:, 0:TT], AF.Silu)
            if "r1" in mixers:
                P.memset(r_y[:, :, :], 1.0)
            for bi in range(TT // BL if "r1" not in mixers else 0):
                c0 = bi * BL
                psv = P.ps()
                for kc in range(8):
                    P.mm(psv[:, 0:256], hT[:, kc, c0:c0 + BL], wA[:, kc, 0:256], start=(kc == 0), stop=(kc == 7))
                P.cp(r_vtok[:, :], psv[:, 0:256], eng="act")
                if "r3" in mixers:
                    continue
                pk = P.pb()
                for p in range(2):
                    P.tr(pk[:, p * 128:(p + 1) * 128], r_k[:, p, c0:c0 + BL], C["ident_b"][:, :])
                P.tt(r_ktok[:, :], pk[:, 0:256], C["ret_kd"][:, :], ALU.mult)
                if "r4" in mixers:
                    continue
                pssA = P.ps()
                pssB = P.ps()
                for h in range(4):
                    p, hh = h // 2, h % 2
                    hp = 64 * hh
                    bank = pssA if hh == 0 else pssB
                    P.mm(bank[:, p * 128:(p + 1) * 128], r_k[hp:hp + 64, p, c0:c0 + BL], r_q[hp:hp + 64, p, c0:c0 + BL])
                for h in range(4):
                    p, hh = h // 2, h % 2
                    bank = pssA if hh == 0 else pssB
                    P.tt(r_sd[:, h * 128:(h + 1) * 128], bank[:, p * 128:(p + 1) * 128], C["ret_mdT"][:, h * 128:(h + 1) * 128], ALU.mult)
                if "r5" in mixers:
                    continue
                psy = P.ps()
                for h in range(4):
                    p, hp = h // 2, 64 * (h % 2)
                    o = psy[hp:hp + 64, p * 128:(p + 1) * 128]
                    P.mm(o, r_vtok[:, h * 64:(h + 1) * 64], r_sd[:, h * 128:(h + 1) * 128], start=True, stop=False)
                    P.mm(o, r_Sb[:, p * 128 + hp:p * 128 + hp + 64], r_qd[:, p, c0:c0 + BL], start=False, stop=True)
                for p in range(2):
                    P.cp(r_y[:, p, c0:c0 + BL], psy[:, p * 128:(p + 1) * 128], eng="act")
                psS = P.ps()
                for h in range(4):
                    p, hp = h // 2, 64 * (h % 2)
                    P.mm(psS[hp:hp + 64, p * 128 + hp:p * 128 + hp + 64], r_ktok[:, h * 64:(h + 1) * 64], r_vtok[:, h * 64:(h + 1) * 64])
                P.tt(r_S[:, :], r_S[:, :], C["ret_cd"][:, :], ALU.mult)
                for h in range(4):
                    p, hp = h // 2, 64 * (h % 2)
                    cs = slice(p * 128 + hp, p * 128 + hp + 64)
                    P.tt(r_S[hp:hp + 64, cs], r_S[hp:hp + 64, cs], psS[hp:hp + 64, cs], ALU.add)
                P.cp(r_Sb[:, :], r_S[:, :], eng="act")
            for p in range(2):
                head_norm_pair(r_y[:, p, :], PV[:, l, 64 + p:65 + p], None, r_gs[:, p, :], mixT[:, p, :])
        else:
            for p in range(2):
                P.memset(mixT[:, p, :], 0.0)
        if "rwkv" in mixers:
            rwkv_tile(b, l, ti)
        else:
            for p in range(2, 5):
                P.memset(mixT[:, p, :], 0.0)
        if "ml" in mixers:
            mlstm_tile(b, l, ti)
        else:
            for p in range(5, 8):
                P.memset(mixT[:, p, :], 0.0)
        for oc in range(8):
            wt = wbuf[wcnt[0] % 2]
            wcnt[0] += 1
            P.dma("sp", wt[:, :, :], SV(WOs[l, oc]))
            ps = P.ps()
            for kc in range(8):
                P.mm(ps[:, 0:TT], wt[:, kc, :], mixT[:, kc, :], start=(kc == 0), stop=(kc == 7))
            P.stt(xT[ti % 2][:, oc, :], ps[:, 0:TT], modT[:, lb, 16 + oc:17 + oc], xT[ti % 2][:, oc, :], ALU.mult, ALU.add)

    def head_norm_pair(y, gain_col, bonus, gate, out):
        psm = P.ps()
        P.mm(psm[:, 0:TT], C["blk64"][:, :], y, start=True, stop=True)
        P.act(r_ysq[:, :], y, AF.Square)
        psq = P.ps()
        P.mm(psq[:, 0:TT], C["blk64"][:, :], r_ysq[:, :], start=True, stop=True)
        P.cp(r_mean[:, :], psm[:, 0:TT], eng="act")
        P.tt(r_var[:, :], r_mean[:, :], r_mean[:, :], ALU.mult, eng="pool")
        P.tt(r_var[:, :], psq[:, 0:TT], r_var[:, :], ALU.subtract)
        rsqrt_eps(r_var[:, :], r_var[:, :], GN_EPS)
        P.tt(r_ysq[:, :], y, r_mean[:, :], ALU.subtract)
        P.stt(r_ysq[:, :], r_ysq[:, :], gain_col, r_var[:, :], ALU.mult, ALU.mult)
        if bonus is not None:
            P.tt(r_ysq[:, :], r_ysq[:, :], bonus, ALU.add, eng="pool")
        P.tt(out, r_ysq[:, :], gate, ALU.mult)

    m_raw = P.sb("m_raw", [128, 8, 3 + TT], F32)
    m_acc = P.sb("m_acc", [128, TT], F32)
    m_q = P.sb("m_q", [128, 4, TT], BF16)
    m_k = P.sb("m_k", [128, 4, TT], BF16)
    m_ip = P.sb("m_ip", [128, TT], F32)
    m_lf = P.sb("m_lf", [128, TT], F32)
    m_F = P.sb("m_F", [128, TT], F32)
    m_m = P.sb("m_m", [128, TT], F32)
    m_beta = P.sb("m_beta", [128, TT], F32)
    m_g = P.sb("m_g", [128, TT], F32)
    m_carry_l = [P.sb("m_carry%d" % i, [128, 2], F32) for i in range(L)]
    m_halo_l = [P.sb("m_halo%d" % i, [128, 8, 3], F32) for i in range(L)]
    m_bbh_l = [P.sb("m_bbh%d" % i, [128, 4, 2], F32) for i in range(L)]
    m_bb = P.sb("m_bb", [128, 4, 1 + TT], F32)
    m_nbb = P.sb("m_nbb", [128, 4, 1 + TT], F32)
    m_ngc = P.sb("m_ngc", [128, 4], F32)
    m_emm = P.sb("m_emm", [128, 4], F32)
    m_E = P.sb("m_E", [128, 512], F32)
    m_EM = P.sb("m_EM", [128, 512], F32)
    m_sD = P.sb("m_sD", [128, 512], BF16)
    m_int = P.sb("m_int", [128, 4, 128], F32)
    m_qi = P.sb("m_qi", [128, 4, 128], BF16)
    m_kws = P.sb("m_kws", [128, 4, 128], BF16)
    m_va = P.sb("m_va", [128, 4, 128], BF16)
    m_C_l = [P.sb("m_C%d" % i, [128, 4, 128], F32) for i in range(L)]
    m_Cb_l = [P.sb("m_Cb%d" % i, [128, 4, 128], BF16) for i in range(L)]
    m_dm = P.sb("m_dm", [128, 4], F32)
    m_hc = P.sb("m_hc", [128, 384], F32)
    m_hsq = m_EM[:, 0:384]
    m_st = P.sb("m_st", [128, 16], F32)
    m_so = P.sb("m_so", [128, 384], F32)
    m_mx = P.sb("m_mx", [128, 384], BF16)
    P.memset(m_kws[:, :, :], 0.0)
    for t_ in (m_ip, m_lf, m_F, m_m, m_beta, m_g):
        P.memset(t_[:, :], 0.0)

    def mlstm_tile(b, l, ti):
        m_carry, m_C, m_Cb = m_carry_l[l], m_C_l[l], m_Cb_l[l]
        P.cp(m_raw[:, :, 0:3], m_halo_l[l][:, :, :])
        P.cp(m_bb[:, :, 0:1], m_bbh_l[l][:, :, 0:1])
        P.cp(m_nbb[:, :, 0:1], m_bbh_l[l][:, :, 1:2])
        for b8 in range(8):
            nm = ("mq%d" % b8) if b8 < 4 else ("mk%d" % (b8 - 4))
            ps = projB(l, nm, None)
            P.cp(m_raw[:, b8, 3:3 + TT], ps[:, 0:TT], eng="act")
            pc = 96 + b8 * 5
            P.act(m_acc[:, :], ps[:, 0:TT], AF.Identity, bias=PV[:, l, pc + 4:pc + 5], scale=PV[:, l, pc + 3:pc + 4])
            for tap in range(3):
                P.stt(m_acc[:, :], m_raw[:, b8, tap:tap + TT], PV[:, l, pc + tap:pc + tap + 1], m_acc[:, :], ALU.mult, ALU.add)
            P.cp(m_halo_l[l][:, b8, :], m_raw[:, b8, TT:TT + 3])
            if b8 < 4:
                P.act(m_q[:, b8, :], m_acc[:, :], AF.Silu)
            else:
                P.act(m_k[:, b8 - 4, :], m_acc[:, :], AF.Silu)
        if "m0" in mixers:
            for j in range(3):
                P.memset(mixT[:, 5 + j, :], 0.0)
            return
        import os as _os
        mlv = int(_os.environ.get("MLV", "99"))

        def _bail():
            for j in range(3):
                P.memset(mixT[:, 5 + j, :], 0.0)
        psi = projB(l, "mi", None)
        P.act(m_ip[0:4, :], psi[0:4, 0:TT], AF.Identity, bias=PV[0:4, l, 140:141])
        if mlv <= 1:
            return _bail()
        psf_ = projB(l, "mf", None)
        P.act(m_lf[0:4, :], psf_[0:4, 0:TT], AF.Sigmoid, bias=PV[0:4, l, 141:142])
        if mlv <= 2:
            return _bail()
        P.act(m_lf[0:4, :], m_lf[0:4, :], AF.Ln)
        if mlv <= 3:
            return _bail()
        P.scan(m_F[0:4, :], C["ones_r"][0:4, :], m_lf[0:4, :], (0.0 if "m2" in mixers else m_carry[0:4, 0:1]), ALU.mult, ALU.add)
        P.scan(m_m[0:4, :], m_lf[0:4, :], m_ip[0:4, :], (0.0 if "m2" in mixers else m_carry[0:4, 1:2]), (ALU.mult if "m3" in mixers else ALU.add), (ALU.add if "m3" in mixers else ALU.max))
        if mlv <= 4:
            return _bail()
        P.tt(m_beta[0:4, :], m_F[0:4, :], m_m[0:4, :], ALU.subtract)
        P.tt(m_g[0:4, :], m_F[0:4, :], m_ip[0:4, :], ALU.subtract)
        P.cp(m_carry[0:4, 0:1], m_F[0:4, TT - 1:TT])
        P.cp(m_carry[0:4, 1:2], m_m[0:4, TT - 1:TT])
        if mlv <= 5:
            return _bail()
        for h in range(4):
            ps = P.ps()
            P.mm(ps[:, 0:TT], (C["blk1"][:, :] if mlv == 6 else C["sel4"][:, h, :]), m_beta[:, :], start=True, stop=True)
            if mlv == 7:
                continue
            P.cp(m_bb[:, h, 1:1 + TT], ps[:, 0:TT], eng="act")
            if mlv == 8:
                continue
            P.ts(m_nbb[:, h, 1:1 + TT], ps[:, 0:TT], -1.0, ALU.mult)
        if "m1" in mixers:
            for j in range(3):
                P.memset(mixT[:, 5 + j, :], 0.0)
        for bi in range(TT // BL if "m1" not in mixers else 0):
            c0 = bi * BL
            pst = P.ps()
            P.mm(pst[:, 0:4], m_g[:, c0:c0 + BL], C["ident_f"][:, 0:4])
            P.mm(pst[:, 4:8], m_m[:, c0:c0 + BL], C["ident_f"][:, 0:4])
            P.ts(m_ngc[:, :], pst[:, 0:4], -1.0, ALU.mult)
            P.act(m_emm[:, :], pst[:, 4:8], AF.Exp, scale=-1.0)
            psv = P.ps()
            for kc in range(8):
                P.mm(psv[:, 0:384], hT[:, kc, c0:c0 + BL], wA[:, kc, 256:640], start=(kc == 0), stop=(kc == 7))
            for h in range(4):
                P.cp(m_va[:, h, 0:96], psv[:, h * 96:(h + 1) * 96], eng="act")
            pso = P.ps()
            for kc in range(8):
                P.mm(pso[:, 0:384], hT[:, kc, c0:c0 + BL], wA[:, kc, 640:1024], start=(kc == 0), stop=(kc == 7))
            P.act(m_so[:, :], pso[:, 0:384], AF.Sigmoid)
            pss_ = P.ps()
            for h in range(4):
                P.mm(pss_[:, h * 128:(h + 1) * 128], m_k[:, h, c0:c0 + BL], m_q[:, h, c0:c0 + BL])
            for h in range(4):
                P.act(m_E[:, h * 128:(h + 1) * 128], m_bb[:, h, 1 + c0:1 + c0 + BL], AF.Exp, bias=m_ngc[:, h:h + 1])
                P.act(m_int[:, h, :], m_bb[:, h, 1 + c0:1 + c0 + BL], AF.Exp, bias=m_nbb[:, h, c0:c0 + 1])
            P.tt(m_EM[:, :], m_E[:, :], C["m_ml4"][:, :], ALU.mult)
            P.tt(m_sD[:, :], pss_[:, :], m_EM[:, :], ALU.mult)
            P.tt(m_qi[:, :, :], m_q[:, :, c0:c0 + BL], m_int[:, :, :], ALU.mult)
            pk = P.ps()
            for h in range(4):
                P.mm(pk[:, h * 96:(h + 1) * 96], m_k[:, h, c0:c0 + BL], C["ident_b"][:, 0:96])
            for h in range(4):
                P.ts(m_kws[:, h, 0:96], pk[:, h * 96:(h + 1) * 96], m_E[:, h * 128 + 127:h * 128 + 128], ALU.mult, 96.0 ** -0.5, ALU.mult)
            psn = P.ps()
            for h in range(4):
                P.mm(psn[:, h * 128:h * 128 + 97], m_sD[:, h * 128:(h + 1) * 128], m_va[:, h, 0:97], start=True, stop=False)
                P.mm(psn[:, h * 128:h * 128 + 97], m_qi[:, h, :], m_Cb[:, h, 0:97], start=False, stop=True)
            psC = P.ps()
            for h in range(4):
                P.mm(psC[:, h * 128:h * 128 + 97], m_kws[:, h, :], m_va[:, h, 0:97])
            for h in range(4):
                P.stt(m_C[:, h, 0:97], m_C[:, h, 0:97], m_int[:, h, 127:128], psC[:, h * 128:h * 128 + 97], ALU.mult, ALU.add)
            P.cp(m_Cb[:, :, :], m_C[:, :, :], eng="act")
            for h in range(4):
                P.cp(m_dm[:, h:h + 1], psn[:, h * 128 + 96:h * 128 + 97])
            P.ts(m_st[:, 12:16], m_dm[:, :], -1.0, ALU.mult)
            P.tt(m_dm[:, :], m_dm[:, :], m_st[:, 12:16], ALU.max)
            P.tt(m_dm[:, :], m_dm[:, :], m_emm[:, :], ALU.max)
            P.recip(m_dm[:, :], m_dm[:, :])
            for h in range(4):
                P.ts(m_hc[:, h * 96:(h + 1) * 96], psn[:, h * 128:h * 128 + 96], m_dm[:, h:h + 1], ALU.mult)
            P.red(m_st[:, 0:4], m_hc[:, :].rearrange("p (h e) -> p h e", h=4))
            P.tt(m_hsq, m_hc[:, :], m_hc[:, :], ALU.mult)
            P.red(m_st[:, 4:8], m_hsq.rearrange("p (h e) -> p h e", h=4))
            P.ts(m_st[:, 0:4], m_st[:, 0:4], 1.0 / 96.0, ALU.mult)
            P.tt(m_st[:, 8:12], m_st[:, 0:4], m_st[:, 0:4], ALU.mult)
            P.stt(m_st[:, 4:8], m_st[:, 4:8], 1.0 / 96.0, m_st[:, 8:12], ALU.mult, ALU.subtract)
            rsqrt_eps(m_st[:, 4:8], m_st[:, 4:8], GN_EPS)
            for h in range(4):
                P.ts(m_hc[:, h * 96:(h + 1) * 96], m_hc[:, h * 96:(h + 1) * 96], m_st[:, h:h + 1], ALU.subtract, m_st[:, 4 + h:5 + h], ALU.mult)
            P.tt(m_mx[:, :], m_hc[:, :], m_so[:, :], ALU.mult)
            pm = P.ps()
            for j in range(3):
                P.mm(pm[:, j * 128:(j + 1) * 128], m_mx[:, j * 128:(j + 1) * 128], C["ident_b"][:, :])
            for j in range(3):
                P.act(mixT[:, 5 + j, c0:c0 + BL], pm[:, j * 128:(j + 1) * 128], AF.Identity, scale=PV[:, l, 142 + j:143 + j])
        P.cp(m_bbh_l[l][:, :, 0:1], m_bb[:, :, TT:TT + 1])
        P.cp(m_bbh_l[l][:, :, 1:2], m_nbb[:, :, TT:TT + 1])

    w_d = P.sb("w_d", [128, TT], F32)
    w_tw = P.sb("w_tw", [128, TT], BF16)
    w_sg = P.sb("w_sg", [128, TT], BF16)
    w_a = P.sb("w_a", [128, 3, TT], F32)
    w_g = P.sb("w_g", [128, 3, TT], F32)
    w_ld = P.sb("w_ld", [128, 3, TT], F32)
    w_cum = P.sb("w_cum", [128, 3, TT], F32)
    w_kk = P.sb("w_kk", [128, 3, TT], F32)
    w_kp = P.sb("w_kp", [128, 3, TT], F32)
    w_bp = P.sb("w_bp", [128, 3, TT], F32)
    w_bon = P.sb("w_bon", [128, 3, TT], F32)
    w_e = P.sb("w_e", [128, TT], F32)
    w_rT = P.sb("w_rT", [128, 3, TT], BF16)
    w_aT = P.sb("w_aT", [128, 3, TT], BF16)
    w_bT = P.sb("w_bT", [128, 3, TT], BF16)
    w_kT = P.sb("w_kT", [128, 3, TT], BF16)
    w_bh = P.sb("w_bh", [128, 3, TT], BF16)
    w_kh = P.sb("w_kh", [128, 3, TT], BF16)
    w_vb = P.sb("w_vb", [128, 3, TT], BF16)
    w_WL = P.sb("w_WL", [128, 3, TT // BL], F32)
    w_S_l = [P.sb("w_S%d" % i, [128, 3, 128], F32) for i in range(L)]
    w_halo_l = [P.sb("w_halo%d" % i, [128, 11], F32) for i in range(L)]
    w_Sb_l = [P.sb("w_Sb%d" % i, [128, 3, 128], BF16) for i in range(L)]
    w_vtok = P.sb("w_vtok", [128, 384], BF16)
    w_bhtok = P.sb("w_bhtok", [128, 384], BF16)
    w_khtok = P.sb("w_khtok", [128, 384], BF16)
    w_AM = [P.sb("w_AM%d" % h, [128, 512], BF16) for h in range(6)]
    w_P = [P.sb("w_P%d" % i, [128, 384], F32) for i in range(4)]
    w_Q = [P.sb("w_Q%d" % i, [128, 384], F32) for i in range(4)]
    w_T = [P.sb("w_T%d" % i, [128, 384], F32) for i in range(2)]
    w_X = P.sb("w_X", [128, 384], F32)
    w_aTf = P.sb("w_aTf", [128, 3, TT], F32)
    w_bTf = P.sb("w_bTf", [128, 3, TT], F32)
    w_U = P.sb("w_U", [128, 384], BF16)
    w_y = P.sb("w_y", [128, 3, TT], F32)

    def rwkv_tile(b, l, ti):
        w_S, w_Sb = w_S_l[l], w_Sb_l[l]
        for j in range(11):
            P.cp(w_raw[:, j, 0:1], w_halo_l[l][:, j:j + 1])
        names = ["wr0", "wr1", "wr2", "wk0", "wk1", "wk2", "wv0", "wv1", "wv2", "wwa", "wgl"]
        for j, nm in enumerate(names):
            ps = projB(l, nm, None)
            P.cp(w_raw[:, j, 1:1 + TT], ps[:, 0:TT], eng="act")
            P.tt(w_d[:, :], w_raw[:, j, 0:TT], w_raw[:, j, 1:1 + TT], ALU.subtract)
            P.stt(w_sh[:, j, :], w_d[:, :], PV[:, l, 69 + j:70 + j], w_raw[:, j, 1:1 + TT], ALU.mult, ALU.add)
            P.cp(w_halo_l[l][:, j:j + 1], w_raw[:, j, TT:TT + 1])
        R = lambda p: w_sh[:, p, :]
        K = lambda p: w_sh[:, 3 + p, :]
        Vv = lambda p: w_sh[:, 6 + p, :]
        P.act(w_tw[0:64, :], w_sh[0:64, 9, :], AF.Tanh)
        P.cp(w_tw[64:128, :], w_sh[64:128, 9, :], eng="act")
        P.act(w_sg[:, :], w_sh[:, 10, :], AF.Sigmoid)
        for p in range(3):
            ps = P.ps()
            P.mm(ps[:, 0:TT], RWL[:, l, p * 128:(p + 1) * 128], w_tw[:, :])
            P.act(w_ld[:, p, :], ps[:, 0:TT], AF.Sigmoid, bias=PV[:, l, 80 + p:81 + p])
            P.ts(w_ld[:, p, :], w_ld[:, p, :], -math.exp(-0.5), ALU.mult)
            ps2 = P.ps()
            P.mm(ps2[:, 0:TT], RWL[:, l, 384 + p * 128:384 + (p + 1) * 128], w_tw[:, :])
            P.act(w_a[:, p, :], ps2[:, 0:TT], AF.Sigmoid, bias=PV[:, l, 83 + p:84 + p])
            ps3 = P.ps()
            P.mm(ps3[:, 0:TT], RWL[:, l, 768 + p * 128:768 + (p + 1) * 128], w_sg[:, :])
            P.cp(w_g[:, p, :], ps3[:, 0:TT], eng="act")
        for p in range(3):
            P.ts(w_kk[:, p, :], K(p), PV[:, l, 86 + p:87 + p], ALU.mult)
            P.tt(w_e[:, :], w_kk[:, p, :], w_kk[:, p, :], ALU.mult)
            ps = P.ps()
            P.mm(ps[:, 0:TT], C["blk1"][:, :], w_e[:, :])
            rsqrt_eps(w_e[:, :], ps[:, 0:TT], 1e-24, ALU.max)
            P.tt(w_kk[:, p, :], w_kk[:, p, :], w_e[:, :], ALU.mult)
            P.ts(w_e[:, :], w_a[:, p, :], PV[:, l, 89 + p:90 + p], ALU.mult, omka[:, l, p:p + 1], ALU.add)
            P.tt(w_kp[:, p, :], K(p), w_e[:, :], ALU.mult)
            P.stt(w_e[:, :], R(p), PV[:, l, 92 + p:93 + p], w_kp[:, p, :], ALU.mult, ALU.mult)
            ps = P.ps()
            P.mm(ps[:, 0:TT], C["blk1"][:, :], w_e[:, :])
            P.tt(w_bon[:, p, :], ps[:, 0:TT], Vv(p), ALU.mult)
            P.tt(w_bp[:, p, :], w_kk[:, p, :], w_a[:, p, :], ALU.mult)
            P.scan(w_cum[:, p, :], C["rst"][:, :], w_ld[:, p, :], 0.0, ALU.mult, ALU.add)
            P.act(w_e[:, :], w_cum[:, p, :], AF.Exp)
            P.tt(w_rT[:, p, :], R(p), w_e[:, :], ALU.mult)
            for bi in range(TT // BL):
                P.cp(w_WL[:, p, bi:bi + 1], w_e[:, bi * BL + BL - 1:bi * BL + BL])
            P.tt(w_e[:, :], w_cum[:, p, :], w_ld[:, p, :], ALU.subtract)
            P.act(w_e[:, :], w_e[:, :], AF.Exp)
            P.stt(w_aT[:, p, :], w_kk[:, p, :], -1.0, w_e[:, :], ALU.mult, ALU.mult)
            P.stt(w_aTf[:, p, :], w_kk[:, p, :], -1.0, w_e[:, :], ALU.mult, ALU.mult)
            P.act(w_e[:, :], w_cum[:, p, :], AF.Exp, scale=-1.0)
            P.tt(w_bT[:, p, :], w_bp[:, p, :], w_e[:, :], ALU.mult)
            P.tt(w_bTf[:, p, :], w_bp[:, p, :], w_e[:, :], ALU.mult)
            P.tt(w_kT[:, p, :], w_kp[:, p, :], w_e[:, :], ALU.mult)
            for bi in range(TT // BL):
                c0 = bi * BL
                P.act(w_e[:, c0:c0 + BL], w_cum[:, p, c0:c0 + BL], AF.Exp, bias=w_cum[:, p, c0 + BL - 1:c0 + BL], scale=-1.0)
            P.tt(w_bh[:, p, :], w_bp[:, p, :], w_e[:, :], ALU.mult)
            P.tt(w_kh[:, p, :], w_kp[:, p, :], w_e[:, :], ALU.mult)
            P.cp(w_vb[:, p, :], Vv(p), eng="act")
        for bi in range(TT // BL):
            c0 = bi * BL
            for (src, dst) in ((w_vb, w_vtok), (w_bh, w_bhtok), (w_kh, w_khtok)):
                pk = P.ps()
                for p in range(3):
                    P.mm(pk[:, p * 128:(p + 1) * 128], src[:, p, c0:c0 + BL], C["ident_b"][:, :])
                P.cp(dst[:, :], pk[:, 0:384], eng="act")
            for h in range(6):
                p, hp = h // 2, 64 * (h % 2)
                sl = slice(hp, hp + 64)
                ps = P.ps()
                P.mm(ps[:, 0:128], w_bTf[sl, p, c0:c0 + BL], w_aTf[sl, p, c0:c0 + BL])
                P.mm(ps[:, 128:256], w_bT[sl, p, c0:c0 + BL], w_rT[sl, p, c0:c0 + BL])
                P.mm(ps[:, 256:384], w_kT[sl, p, c0:c0 + BL], w_aT[sl, p, c0:c0 + BL])
                P.mm(ps[:, 384:512], w_kT[sl, p, c0:c0 + BL], w_rT[sl, p, c0:c0 + BL])
                P.tt(w_AM[h][:, 128:512], ps[:, 128:512], C["m_rw"][:, 128:512], ALU.mult)
                P.tt(w_P[2 * (h % 2)][:, p * 128:(p + 1) * 128], ps[:, 0:128], C["m_rw"][:, 0:128], ALU.mult)
            TTm = {}
            for g in range(2):
                sl = slice(64 * g, 64 * g + 64)
                psq = P.ps()
                for j in range(3):
                    P.mm(psq[:, j * 128:(j + 1) * 128], w_aTf[sl, j, c0:c0 + BL], w_bTf[sl, j, c0:c0 + BL])
                Pm, Qm, Tm = w_P[2 * g], w_Q[2 * g], w_T[g]
                for j in range(3):
                    P.tt(Qm[:, j * 128:(j + 1) * 128], psq[:, j * 128:(j + 1) * 128], C["m_lo"][:, :], ALU.mult)
                for j in range(3):
                    P.tt(Tm[:, j * 128:(j + 1) * 128], Pm[:, j * 128:(j + 1) * 128], C["ident_f"][:, :], ALU.add)
                cur = 0
                for lev in range(6):
                    Pn, Qn = w_P[2 * g + 1 - cur], w_Q[2 * g + 1 - cur]
                    Po, Qo = w_P[2 * g + cur], w_Q[2 * g + cur]
                    pp = P.ps()
                    pq = P.ps()
                    for j in range(3):
                        P.mm(pp[:, j * 128:(j + 1) * 128], Qo[:, j * 128:(j + 1) * 128], Po[:, j * 128:(j + 1) * 128])
                    for j in range(3):
                        P.mm(pq[:, j * 128:(j + 1) * 128], Po[:, j * 128:(j + 1) * 128], Qo[:, j * 128:(j + 1) * 128])
                    P.cp(Pn[:, :], pp[:, 0:384], eng="act")
                    P.cp(Qn[:, :], pq[:, 0:384])
                    pt = P.ps()
                    for j in range(3):
                        P.mm(pt[:, j * 128:(j + 1) * 128], Qn[:, j * 128:(j + 1) * 128], Tm[:, j * 128:(j + 1) * 128])
                    P.tt(Tm[:, :], Tm[:, :], pt[:, 0:384], ALU.add)
                    cur = 1 - cur
                TTm[g] = Tm
            psX = P.ps()
            for h in range(6):
                p, hp = h // 2, 64 * (h % 2)
                P.mm(psX[:, h * 64:(h + 1) * 64], w_aT[:, p, c0:c0 + BL], w_Sb[:, p, hp:hp + 64], start=True, stop=False)
                P.mm(psX[:, h * 64:(h + 1) * 64], w_AM[h][:, 256:384], w_vtok[:, h * 64:(h + 1) * 64], start=False, stop=True)
            P.cp(w_X[:, :], psX[:, 0:384], eng="act")
            psU = P.ps()
            for h in range(6):
                g, j = h % 2, h // 2
                P.mm(psU[:, h * 64:(h + 1) * 64], TTm[g][:, j * 128:(j + 1) * 128], w_X[:, h * 64:(h + 1) * 64])
            P.cp(w_U[:, :], psU[:, 0:384], eng="act")
            psY = P.ps()
            for h in range(6):
                p, hp = h // 2, 64 * (h % 2)
                o = psY[hp:hp + 64, p * 128:(p + 1) * 128]
                P.mm(o, w_Sb[:, p, hp:hp + 64], w_rT[:, p, c0:c0 + BL], start=True, stop=False)
                P.mm(o, w_U[:, h * 64:(h + 1) * 64], w_AM[h][:, 128:256], start=False, stop=False)
                P.mm(o, w_vtok[:, h * 64:(h + 1) * 64], w_AM[h][:, 384:512], start=False, stop=True)
            for p in range(3):
                P.cp(w_y[:, p, c0:c0 + BL], psY[:, p * 128:(p + 1) * 128], eng="act")
            psS = P.ps()
            for h in range(6):
                p, hp = h // 2, 64 * (h % 2)
                o = psS[hp:hp + 64, p * 128 + hp:p * 128 + hp + 64]
                P.mm(o, w_bhtok[:, h * 64:(h + 1) * 64], w_U[:, h * 64:(h + 1) * 64], start=True, stop=False)
                P.mm(o, w_khtok[:, h * 64:(h + 1) * 64], w_vtok[:, h * 64:(h + 1) * 64], start=False, stop=True)
            for h in range(6):
                p, hp = h // 2, 64 * (h % 2)
                P.stt(w_S[hp:hp + 64, p, hp:hp + 64], w_S[hp:hp + 64, p, hp:hp + 64], w_WL[hp:hp + 64, p, bi:bi + 1],
                      psS[hp:hp + 64, p * 128 + hp:p * 128 + hp + 64], ALU.mult, ALU.add)
            P.cp(w_Sb[:, :, :], w_S[:, :, :], eng="act")
        for p in range(3):
            head_norm_pair(w_y[:, p, :], PV[:, l, 66 + p:67 + p], w_bon[:, p, :], w_g[:, p, :], mixT[:, 2 + p, :])

    def layer_tile_ffn(b, l, ti):
        lb = l * BPC + b
        norm_mod(ti, lambda kc: gmodf[:, lb, kc:kc + 1], lambda kc: modT[:, lb, 24 + kc:25 + kc])
        for fb in range(NFB):
            wt = w1buf[fb % 2]
            P.dma("act" if fb % 2 else "sp", wt[:, :, :], SV(W1s[l, fb]))
            psg = P.ps()
            psu = P.ps()
            for kc in range(8):
                P.mm(psg[:, 0:TT], wt[:, kc, 0:128], hT[:, kc, :], start=(kc == 0), stop=(kc == 7))
            for kc in range(8):
                P.mm(psu[:, 0:TT], wt[:, kc, 128:256], hT[:, kc, :], start=(kc == 0), stop=(kc == 7))
            sg_ = sgt[fb % 2]
            P.act(sg_[:, :], psg[:, 0:TT], AF.Silu)
            P.tt(actT[:, fb, :], sg_[:, :], psu[:, 0:TT], ALU.mult)
        for oc in range(8):
            wt = w2buf[oc % 2]
            P.dma("act" if oc % 2 else "sp", wt[:, :, :], SV(W2s[l, oc]))
            ps = P.ps()
            for fb in range(NFB):
                P.mm(ps[:, 0:TT], wt[:, fb, :], actT[:, fb, :], start=(fb == 0), stop=(fb == NFB - 1))
            P.stt(xT[ti % 2][:, oc, :], ps[:, 0:TT], modT[:, lb, 40 + oc:41 + oc], xT[ti % 2][:, oc, :], ALU.mult, ALU.add)

    final_tokens = []
    gti = 0
    for b in range(BPC):
        if b > 0:
            P.barrier()
        for l in range(L):
            P.memset(r_S_l[l][:, :], 0.0)
            P.memset(r_Sb_l[l][:, :], 0.0)
            P.memset(m_halo_l[l][:, :, :], 0.0)
            P.memset(m_carry_l[l][:, :], 0.0)
            P.memset(m_bbh_l[l][:, :, :], 0.0)
            P.memset(m_C_l[l][:, :, :], 0.0)
            P.memset(m_Cb_l[l][:, :, :], 0.0)
            P.memset(w_halo_l[l][:, :], 0.0)
            P.memset(w_S_l[l][:, :, :], 0.0)
            P.memset(w_Sb_l[l][:, :, :], 0.0)
        P.memset(m_va[:, :, 96:97], 1.0)
        for ti in range(NT):
            x = xT[ti % 2]
            P.dma("sp", x[:, :, :], x_d[b, :, :, ti * TT:(ti + 1) * TT])
            P.dma("sp", ropeb[ti % 2][:, :, :], rope_d[:, :, ti * TT:(ti + 1) * TT])
            for l in range(L):
                P.dma("act", wA[:, :, :], SV(WAs[l]))
                if "nomix" not in mixers:
                    layer_tile_mixer(b, l, ti)
                if "noffn" not in mixers:
                    layer_tile_ffn(b, l, ti)
            psm = P.ps()
            for kc in range(8 if "nofinal" not in mixers else 0):
                sq_ = sq[kc % 2]
                P.act(sq_[:, :], x[:, kc, :], AF.Square)
                P.mm(psm[:, 0:TT], C["onesN"][:, :], sq_[:, :], start=(kc == 0), stop=(kc == 7))
            if "nofinal" not in mixers:
                rsqrt_eps(rstd[:, :], psm[:, 0:TT], RMS_EPS)
            for kc in range(8 if ("nofinal" not in mixers and "f3" not in mixers) else 0):
                P.stt(x[:, kc, :], x[:, kc, :], FN[:, kc:kc + 1], rstd[:, :], ALU.mult, ALU.mult)
            final_tokens.append(P.dma("sp", y_d[b, :, :, ti * TT:(ti + 1) * TT], x[:, :, :]))
    P.emit(final_tokens)
    es.close()
    return nc, consts, rope_np


_CACHE = {}


def run(inputs, n_cores, BPC, T, L, mixers=("ret", "rwkv", "ml"), lay=None):
    key = (BPC, T, L, tuple(mixers))
    if key not in _CACHE:
        _CACHE[key] = build_program(BPC, T, L, mixers)
    nc, consts, rope_np = _CACHE[key]
    if lay is None:
        lay = build_layer_inputs(inputs, L)
    x = np.asarray(inputs["x"], np.float32)
    c = np.asarray(inputs["c"], np.float32)
    in_maps = []
    for core in range(n_cores):
        xs = x[core * BPC:(core + 1) * BPC]
        xTm = np.ascontiguousarray(xs.reshape(BPC, T, 8, 128).transpose(0, 3, 2, 1))
        cs = c[core * BPC:(core + 1) * BPC]
        cTm = np.ascontiguousarray(cs.reshape(BPC, 8, 128).transpose(2, 1, 0))
        m = {"x": xTm, "cT": cTm, "rope": rope_np}
        m.update(lay)
        for k, v in consts.items():
            m["c_" + k] = v
        in_maps.append(m)
    res = run_bass_kernel_spmd(nc, in_maps, core_ids=list(range(n_cores)))
    outs = []
    for core in range(n_cores):
        yT = res.results[core]["y"]
        outs.append(np.ascontiguousarray(yT.transpose(0, 3, 2, 1)).reshape(BPC, T, 1024))
    return np.concatenate(outs, 0).astype(np.float32)


def kernel(**inputs):
    B, T, _ = inputs["x"].shape
    L = inputs["w_in"].shape[0]
    return run(inputs, 8, B // 8, T, L)
```
